# Optimizing a Trainium2 kernel written in Bass

```python
import jax, jax.numpy as jnp
from jax import lax
import numpy as np

D_MODEL = 1024
BATCH = 4
SEQ = 4096
DEPTH = 1

RET_WIDTH = D_MODEL // 2
RET_HEADS = 4
RET_HEAD_DIM = RET_WIDTH // RET_HEADS
CONV_WIDTH = D_MODEL - RET_WIDTH
CONV_GROUPS = 8
CONV_K = 3
MIX_WIDTH = RET_WIDTH + CONV_WIDTH
IN_COLS = 4 * RET_WIDTH + 3 * CONV_WIDTH
CHUNK = 128
ROPE_BASE = 10000.0
N_GROUPS = 8
EXPERTS_PER_GROUP = 8
N_EXPERTS = N_GROUPS * EXPERTS_PER_GROUP
TOP_K = 2
EXPERT_FF = D_MODEL // 2
MOE_BLOCK = 128
EPS = 1e-6

kernel_name = "hybrid_retention_shortconv_hiermoe"


def rmsnorm(x, w):
    xf = x.astype(jnp.float32)
    y = xf * lax.rsqrt(jnp.mean(xf * xf, axis=-1, keepdims=True) + EPS)
    return (y * w.astype(jnp.float32)).astype(x.dtype)


def rotary(x, pos):
    half = x.shape[-1] // 2
    inv = ROPE_BASE ** (-jnp.arange(half, dtype=jnp.float32) / half)
    ang = pos.astype(jnp.float32)[:, None] * inv[None, :]
    cos = jnp.cos(ang)[None, :, None, :]
    sin = jnp.sin(ang)[None, :, None, :]
    xf = x.astype(jnp.float32)
    x1, x2 = xf[..., :half], xf[..., half:]
    return jnp.concatenate([x1 * cos - x2 * sin, x2 * cos + x1 * sin], axis=-1)


def retention_chunkwise(q, k, v):
    b, s, h, d = q.shape
    nc = s // CHUNK
    log_g = jnp.log1p(-(2.0 ** (-5.0 - jnp.arange(h, dtype=jnp.float32))))
    idx = jnp.arange(CHUNK, dtype=jnp.float32)
    rel = idx[:, None] - idx[None, :]
    causal = rel >= 0
    intra_decay = jnp.where(causal[None], jnp.exp(log_g[:, None, None] * jnp.where(causal, rel, 0.0)[None]), 0.0)
    k_decay = jnp.exp(log_g[:, None] * (CHUNK - 1 - idx)[None, :])
    q_decay = jnp.exp(log_g[:, None] * (idx + 1)[None, :])
    chunk_decay = jnp.exp(log_g * CHUNK)

    qc = q.reshape(b, nc, CHUNK, h, d)
    kc = k.reshape(b, nc, CHUNK, h, d)
    vc = v.reshape(b, nc, CHUNK, h, d)

    scores = jnp.einsum('bnihd,bnjhd->bnhij', qc, kc) * intra_decay[None, None]
    o_intra = jnp.einsum('bnhij,bnjhd->bnihd', scores, vc)

    kv = jnp.einsum('bnjhd,hj,bnjhe->bnhde', kc, k_decay, vc)

    def step(state, kv_n):
        return chunk_decay[None, :, None, None] * state + kv_n, state

    _, prev = lax.scan(step, jnp.zeros((b, h, d, d), jnp.float32), jnp.moveaxis(kv, 1, 0))
    prev = jnp.moveaxis(prev, 0, 1)
    o_inter = jnp.einsum('bnihd,bnhde->bnihe', qc, prev) * q_decay.T[None, None, :, :, None]
    return (o_intra + o_inter).reshape(b, s, h, d)


def head_groupnorm(o, w):
    b, s, h, d = o.shape
    mu = jnp.mean(o, axis=-1, keepdims=True)
    var = jnp.mean(jnp.square(o - mu), axis=-1, keepdims=True)
    y = (o - mu) * lax.rsqrt(var + EPS)
    return y.reshape(b, s, h * d) * w.astype(jnp.float32)


def causal_depthwise_conv(u, w):
    return lax.conv_general_dilated(
        u, w[:, None, :], window_strides=(1,), padding=[(CONV_K - 1, 0)],
        dimension_numbers=('NWC', 'WIO', 'NWC'), feature_group_count=u.shape[-1])


def mixer(h, w_in, ret_gn_w, conv_w, w_o):
    b, s, _ = h.shape
    proj = h @ w_in
    R, C = RET_WIDTH, CONV_WIDTH
    q, k, v, g, gb, gc, u = jnp.split(proj, [R, 2 * R, 3 * R, 4 * R, 4 * R + C, 4 * R + 2 * C], axis=-1)
    pos = jnp.arange(s)
    q = rotary(q.reshape(b, s, RET_HEADS, RET_HEAD_DIM), pos) * (RET_HEAD_DIM ** -0.5)
    k = rotary(k.reshape(b, s, RET_HEADS, RET_HEAD_DIM), pos)
    v = v.reshape(b, s, RET_HEADS, RET_HEAD_DIM).astype(jnp.float32)
    ret = retention_chunkwise(q, k, v)
    ret = (jax.nn.silu(g.astype(jnp.float32)) * head_groupnorm(ret, ret_gn_w)).astype(h.dtype)
    conv = gb * causal_depthwise_conv(gc * u, conv_w)
    return jnp.concatenate([ret, conv], axis=-1) @ w_o


def expert_dispatch(t, expert, gate, w_gate, w_up, w_down):
    n, d = t.shape
    e_total = w_gate.shape[0]
    n_slots = n * TOP_K
    flat_e = expert.reshape(-1)
    flat_tok = jnp.repeat(jnp.arange(n, dtype=jnp.int32), TOP_K)
    flat_w = gate.reshape(-1)
    order = jnp.argsort(flat_e)
    se, stok, sw = flat_e[order], flat_tok[order], flat_w[order]
    counts = jnp.bincount(flat_e, length=e_total)
    starts = jnp.cumsum(counts) - counts
    padded = ((counts + MOE_BLOCK - 1) // MOE_BLOCK) * MOE_BLOCK
    pad_ends = jnp.cumsum(padded)
    pad_starts = pad_ends - padded
    dest = pad_starts[se] + (jnp.arange(n_slots) - starts[se])
    n_blocks = -(-n_slots // MOE_BLOCK) + e_total
    cap = n_blocks * MOE_BLOCK
    buf_tok = jnp.full((cap,), n, jnp.int32).at[dest].set(stok)
    buf_w = jnp.zeros((cap,), t.dtype).at[dest].set(sw.astype(t.dtype))
    block_e = jnp.minimum(jnp.searchsorted(pad_ends, jnp.arange(n_blocks) * MOE_BLOCK, side='right'), e_total - 1)
    t_pad = jnp.concatenate([t, jnp.zeros((1, d), t.dtype)], axis=0)

    def run_block(args):
        tok, e = args
        xb = t_pad[tok]
        hb = jax.nn.silu(xb @ w_gate[e]) * (xb @ w_up[e])
        return hb @ w_down[e]

    yb = lax.map(run_block, (buf_tok.reshape(n_blocks, MOE_BLOCK), block_e))
    y = jax.ops.segment_sum(yb.reshape(cap, d) * buf_w[:, None], buf_tok, num_segments=n + 1)
    return y[:n]


def hier_moe(h, rg_w, rg_b, re_w, re_b, w_gate, w_up, w_down):
    b, s, d = h.shape
    t = h.reshape(b * s, d)
    g_prob = jax.nn.softmax((t @ rg_w).astype(jnp.float32) + rg_b.astype(jnp.float32), axis=-1)
    p_group, g_sel = lax.top_k(g_prob, 1)
    p_group, g_sel = p_group[:, 0], g_sel[:, 0]
    e_logits_all = jnp.einsum('nd,gde->nge', t, re_w).astype(jnp.float32) + re_b.astype(jnp.float32)[None]
    e_logits = jnp.take_along_axis(e_logits_all, g_sel[:, None, None], axis=1)[:, 0]
    e_prob = jax.nn.softmax(e_logits, axis=-1)
    top_p, top_i = lax.top_k(e_prob, TOP_K)
    top_p = top_p / jnp.sum(top_p, axis=-1, keepdims=True)
    gate = p_group[:, None] * top_p
    expert = g_sel[:, None] * EXPERTS_PER_GROUP + top_i
    return expert_dispatch(t, expert, gate, w_gate, w_up, w_down).reshape(b, s, d)


def setup_inputs(seed: int = 0) -> dict:
    key = jax.random.key(seed)
    ks = jax.random.split(key, 16)
    f32 = jnp.float32
    nrm = lambda k, shape, scale: jax.random.normal(k, shape, f32) * scale
    return {
        "x": nrm(ks[0], (BATCH, SEQ, D_MODEL), 1.0),
        "norm1_w": 1.0 + nrm(ks[1], (DEPTH, D_MODEL), 0.02),
        "w_in": nrm(ks[2], (DEPTH, D_MODEL, IN_COLS), D_MODEL ** -0.5),
        "ret_gn_w": 1.0 + nrm(ks[3], (DEPTH, RET_WIDTH), 0.02),
        "conv_w": nrm(ks[4], (DEPTH, CONV_K, CONV_WIDTH), CONV_K ** -0.5),
        "w_o": nrm(ks[5], (DEPTH, MIX_WIDTH, D_MODEL), MIX_WIDTH ** -0.5),
        "norm2_w": 1.0 + nrm(ks[6], (DEPTH, D_MODEL), 0.02),
        "router_g_w": nrm(ks[7], (DEPTH, D_MODEL, N_GROUPS), D_MODEL ** -0.5),
        "router_g_b": nrm(ks[8], (DEPTH, N_GROUPS), 0.01),
        "router_e_w": nrm(ks[9], (DEPTH, N_GROUPS, D_MODEL, EXPERTS_PER_GROUP), D_MODEL ** -0.5),
        "router_e_b": nrm(ks[10], (DEPTH, N_GROUPS, EXPERTS_PER_GROUP), 0.01),
        "w_gate": nrm(ks[11], (DEPTH, N_EXPERTS, D_MODEL, EXPERT_FF), D_MODEL ** -0.5),
        "w_up": nrm(ks[12], (DEPTH, N_EXPERTS, D_MODEL, EXPERT_FF), D_MODEL ** -0.5),
        "w_down": nrm(ks[13], (DEPTH, N_EXPERTS, EXPERT_FF, D_MODEL), EXPERT_FF ** -0.5),
        "final_norm_w": 1.0 + nrm(ks[14], (D_MODEL,), 0.02),
    }


def reference(x, norm1_w, w_in, ret_gn_w, conv_w, w_o, norm2_w, router_g_w, router_g_b,
              router_e_w, router_e_b, w_gate, w_up, w_down, final_norm_w):
    for l in range(DEPTH):
        h = rmsnorm(x, norm1_w[l])
        x = x + mixer(h, w_in[l], ret_gn_w[l], conv_w[l], w_o[l])
        h = rmsnorm(x, norm2_w[l])
        x = x + hier_moe(h, router_g_w[l], router_g_b[l], router_e_w[l], router_e_b[l],
                         w_gate[l], w_up[l], w_down[l])
    return rmsnorm(x, final_norm_w)
```

```python
import numpy as np
import ml_dtypes
from contextlib import ExitStack
import concourse.bass as bass
import concourse.mybir as mybir
from concourse.bass_utils import run_bass_kernel_spmd

F32 = mybir.dt.float32
BF16 = mybir.dt.bfloat16
I32 = mybir.dt.int32
AF = mybir.ActivationFunctionType
ALU = mybir.AluOpType
AX = mybir.AxisListType

NCORES = 8
D = 1024
SEQ = 4096
TOK = 2048
NT = TOK // 128
NE = 64
EPS = 1e-6
NEG = -1.0e30

ENGS = ("sync", "act", "pool", "pe", "dve")
NDMASEM = 6


class Op:
    __slots__ = ("eng", "fn", "deps", "dma", "needs_inc", "seq", "sem", "semval", "guard", "selfwait", "raw")

    def __init__(self, eng, fn, deps, dma):
        self.eng = eng
        self.fn = fn
        self.deps = deps
        self.dma = dma
        self.needs_inc = False
        self.seq = 0
        self.sem = None
        self.semval = 0
        self.guard = None
        self.selfwait = False


class Sched:
    def __init__(self, nc):
        self.nc = nc
        self.ops = {e: [] for e in ENGS}
        self.res = {}
        self.dma_hist = {e: [] for e in ENGS}
        self.uid = 0
        self.all_selfwait = True

    @staticmethod
    def _name(x):
        if isinstance(x, str):
            return x
        if hasattr(x, "tensor"):
            return x.tensor.name
        return x.name

    def add(self, eng, fn, reads=(), writes=(), dma=False, acc=False, selfwait=False):
        deps = {}
        raw = set()
        for r in reads:
            st = self.res.get(self._name(r))
            if st is not None:
                for o in st[0].values():
                    deps[id(o)] = o
                    raw.add(id(o))
        for w in writes:
            st = self.res.get(self._name(w))
            if st is not None:
                for o in st[1].values():
                    deps[id(o)] = o
                if not acc:
                    for o in st[0].values():
                        deps[id(o)] = o
                        raw.add(id(o))
                else:
                    for o in st[2].values():
                        deps[id(o)] = o
        op = Op(eng, fn, list(deps.values()), dma)
        op.selfwait = (selfwait or self.all_selfwait) and eng != "pe"
        op.raw = raw
        if dma:
            hist = self.dma_hist[eng]
            i = len(hist)
            if i >= NDMASEM:
                op.guard = hist[i - NDMASEM]
            op.sem = i % NDMASEM
            op.semval = 16 * (i // NDMASEM + 1)
            hist.append(op)
        self.uid += 1
        key = ("dma", self.uid) if dma else eng
        for r in reads:
            st = self.res.setdefault(self._name(r), ({}, {}, {}))
            st[1][key] = op
        for w in writes:
            st = self.res.setdefault(self._name(w), ({}, {}, {}))
            if not acc:
                st[2].clear()
                for k_, o in st[0].items():
                    st[2][("w", k_)] = o
                for k_, o in st[1].items():
                    st[2][("r", k_)] = o
                st[0].clear()
                st[1].clear()
            st[0][key] = op
        self.ops[eng].append(op)
        return op

    def barrier_all(self):
        last = []
        for e in ENGS:
            for o in reversed(self.ops[e]):
                if o.fn is not None and not o.dma:
                    last.append(o)
                    break
            for o in self.dma_hist[e][-NDMASEM:]:
                last.append(o)
        for e in ENGS:
            self.ops[e].append(Op(e, None, list(last), False))
        self.res.clear()

    def emit(self, stack):
        nc = self.nc
        for e in ENGS:
            for op in self.ops[e]:
                for d in op.deps:
                    if d.dma or d.eng != e or (op.selfwait and id(d) in op.raw):
                        d.needs_inc = True
        esem = {e: stack.enter_context(nc.semaphore("s_" + e)) for e in ENGS}
        dsem = {e: [stack.enter_context(nc.semaphore("d_%s%d" % (e, i))) for i in range(NDMASEM)]
                for e in ("sync", "act", "pool")}
        for e in ENGS:
            n = 0
            for op in self.ops[e]:
                if op.dma:
                    continue
                if op.needs_inc:
                    n += 1
                    op.seq = n
        block = stack.enter_context(nc.Block())
        hw = {"sync": block.sync, "act": block.scalar, "pool": block.gpsimd,
              "pe": block.tensor, "dve": block.vector}

        def make(e):
            def body(eng):
                waited = {}

                def wait(sem, key, val):
                    if waited.get(key, 0) >= val:
                        return
                    waited[key] = val
                    eng.wait_ge(sem, val)

                for op in self.ops[e]:
                    for d in op.deps:
                        if d.dma:
                            wait(dsem[d.eng][d.sem], ("d", d.eng, d.sem), d.semval)
                        elif d.eng != e or (op.selfwait and id(d) in op.raw):
                            wait(esem[d.eng], ("e", d.eng), d.seq)
                    if op.guard is not None:
                        g = op.guard
                        wait(dsem[e][g.sem], ("d", e, g.sem), g.semval)
                    if op.fn is None:
                        continue
                    ins = op.fn(eng)
                    if op.dma:
                        ins.then_inc(dsem[e][op.sem], 16)
                    elif op.needs_inc:
                        ins.then_inc(esem[e], 1)
                for o in self.dma_hist[e][-NDMASEM:]:
                    wait(dsem[e][o.sem], ("d", e, o.sem), o.semval)
            return body

        for e in ENGS:
            hw[e](make(e))


class KB:
    def __init__(self, nc, S):
        self.nc = nc
        self.S = S

    def dma(self, q, out, in_, reads=(), writes=(), acc=False):
        return self.S.add(q, lambda e: e.dma_start(out=out, in_=in_), reads=reads, writes=writes, dma=True, acc=acc)

    def mm(self, out, lhsT, rhs, start, stop):
        return self.S.add("pe", lambda e: e.matmul(out, lhsT=lhsT, rhs=rhs, start=start, stop=stop),
                          reads=[lhsT, rhs], writes=[out])

    def tr(self, out, in_, ident):
        return self.S.add("pe", lambda e: e.transpose(out=out, in_=in_, identity=ident),
                          reads=[in_, ident], writes=[out])

    def act(self, out, in_, func, bias=None, scale=None, accum=None, eng="act", acc=False, extra_w=()):
        kw = {}
        rd = [in_]
        if bias is not None:
            kw["bias"] = bias
            if not isinstance(bias, (int, float)):
                rd.append(bias)
        if scale is not None:
            kw["scale"] = scale
            if not isinstance(scale, (int, float)):
                rd.append(scale)
        wr = [out]
        if accum is not None:
            kw["accum_out"] = accum
            wr.append(accum)
        return self.S.add(eng, lambda e: e.activation(out=out, in_=in_, func=func, **kw), reads=rd, writes=wr, acc=acc)

    def tt(self, eng, out, in0, in1, op, acc=False):
        return self.S.add(eng, lambda e: e.tensor_tensor(out=out, in0=in0, in1=in1, op=op),
                          reads=[in0, in1], writes=[out], acc=acc)

    def ts(self, eng, out, in0, s1, op0, s2=None, op1=None, acc=False):
        rd = [in0]
        for s in (s1, s2):
            if s is not None and not isinstance(s, (int, float)):
                rd.append(s)
        if op1 is None:
            fn = lambda e: e.tensor_scalar(out=out, in0=in0, scalar1=s1, scalar2=None, op0=op0)
        else:
            fn = lambda e: e.tensor_scalar(out=out, in0=in0, scalar1=s1, scalar2=s2, op0=op0, op1=op1)
        return self.S.add(eng, fn, reads=rd, writes=[out], acc=acc)

    def stt(self, out, in0, scalar, in1, op0, op1, acc=False):
        rd = [in0, in1]
        if not isinstance(scalar, (int, float)):
            rd.append(scalar)
        return self.S.add("dve", lambda e: e.scalar_tensor_tensor(out=out, in0=in0, scalar=scalar, in1=in1, op0=op0, op1=op1),
                          reads=rd, writes=[out], acc=acc)

    def copy(self, eng, out, in_, acc=False):
        if eng == "act":
            return self.S.add(eng, lambda e: e.copy(out=out, in_=in_), reads=[in_], writes=[out], acc=acc)
        return self.S.add(eng, lambda e: e.tensor_copy(out=out, in_=in_), reads=[in_], writes=[out], acc=acc)

    def red(self, eng, out, in_, op, acc=False):
        return self.S.add(eng, lambda e: e.tensor_reduce(out=out, in_=in_, axis=AX.X, op=op),
                          reads=[in_], writes=[out], acc=acc)

    def recip(self, out, in_):
        return self.S.add("dve", lambda e: e.reciprocal(out=out, in_=in_), reads=[in_], writes=[out])

    def memset(self, eng, ap, val):
        return self.S.add(eng, lambda e: e.memset(ap, val), writes=[ap])


def build_nc(debug=0, phases=4):
    nc = bass.Bass("TRN2", target_bir_lowering=False)
    din = lambda n, s, dt=F32: nc.dram_tensor(n, s, dt, kind="ExternalInput").ap()
    x_own = din("x_own", [TOK, D])
    x_pre = din("x_pre", [TOK, D])
    tab_own = din("tab_own", [TOK, 4, 256])
    tab_pre = din("tab_pre", [TOK, 2, 256])
    w_in = din("w_in", [D, 3584])
    w_o = din("w_o", [D, D])
    if phases >= 3:
        w_gate = din("w_gate", [NE, D, 512])
        w_up = din("w_up", [NE, D, 512])
        w_down = din("w_down", [NE, 512, D])
    wr_d = din("wr", [D, 72])
    c_n1w = din("c_n1w", [128, D])
    c_n2w = din("c_n2w", [128, D])
    c_fnw = din("c_fnw", [128, D])
    c_gnw = din("c_gnw", [128, 512])
    c_rb = din("c_rb", [128, 72])
    c_cw = din("c_cw", [128, 12])
    c_id16 = din("c_id16", [128, 128], BF16)
    c_id32 = din("c_id32", [128, 128])
    c_mask = din("c_mask", [128, 512])
    c_ustr = din("c_ustr", [128, 128])
    c_ones = din("c_ones", [128, 128])
    c_base = din("c_base", [128, 64])
    y_out = nc.dram_tensor("y_out", [TOK, D], F32, kind="ExternalOutput").ap()
    dscr = lambda n, s, dt: nc.dram_tensor(n, s, dt, kind="Internal").ap()
    xs_d = dscr("xs_d", [NE * 128, D], BF16)
    ys_d = dscr("ys_d", [NE * 128, D], F32)
    x1_d = dscr("x1_d", [TOK, D], F32)
    h2_d = dscr("h2_d", [TOK, D], BF16)
    dbg = {}
    if debug:
        dbg["lg"] = nc.dram_tensor("dbg_lg", [128, NT * 72], F32, kind="ExternalOutput").ap()
        dbg["x1"] = nc.dram_tensor("dbg_x1", [TOK, D], F32, kind="ExternalOutput").ap()
        dbg["rt"] = nc.dram_tensor("dbg_rt", [128, 4 * NT], F32, kind="ExternalOutput").ap()
        for nm in ("kh", "vb", "qh", "retb"):
            dbg[nm] = nc.dram_tensor("dbg_" + nm, [TOK, 512], BF16, kind="ExternalOutput").ap()

    REG = {}

    def bnd(e):
        if "r" not in REG:
            REG["r"] = e.alloc_register("bnd")
            e.reg_mov(REG["r"], NE * 128 - 1)
        return REG["r"]

    cd = [float(np.exp(np.log1p(-(2.0 ** (-5.0 - h))) * 128.0)) for h in range(4)]

    with ExitStack() as st:
        S = Sched(nc)
        K = KB(nc, S)
        sb = lambda name, shape, dt, stk=st: stk.enter_context(nc.sbuf_tensor(name, shape, dt))
        ps = lambda name, shape, dt: st.enter_context(nc.psum_tensor(name, shape, dt))
        PB = [ps("PB%d" % i, [128, 1024], BF16) for i in range(2)]
        PF = [ps("PF%d" % i, [128, 512], F32) for i in range(6)]
        id16 = sb("id16", [128, 128], BF16)
        id32 = sb("id32", [128, 128], F32)
        epsb = sb("epsb", [128, 1], F32)
        LG = sb("LG", [128, NT, 72], F32)
        dst = [sb("dst%d" % k, [128, NT], I32) for k in range(2)]
        gw = [sb("gw%d" % k, [128, NT], F32) for k in range(2)]
        xt = [sb("xt%d" % i, [128, D], F32) for i in range(3)]
        junk = sb("junk", [128, D], BF16)
        ssq = sb("ssq", [128, 1], F32)
        rstd = sb("rstd", [128, 1], F32)

        K.dma("sync", id16[:], c_id16, writes=[id16])
        K.dma("sync", id32[:], c_id32, writes=[id32])
        K.memset("dve", epsb[:], EPS)
        negh = sb("negh", [128, 8], F32)
        K.memset("pool", negh[:], -0.5)

        def rsqrt_pool(out, in_, scale, n):
            K.ts("pool", out, in_, scale, ALU.mult, EPS, ALU.add)
            K.tt("pool", out, out, negh[:, 0:n], ALU.pow)

        def rms_stats(src, n=D):
            K.act(junk[:, 0:n], src, AF.Square, accum=ssq[:])
            rsqrt_pool(rstd[:], ssq[:], 1.0 / n, 1)

        with ExitStack() as p1:
            sb1 = lambda name, shape, dt: sb(name, shape, dt, p1)
            winb = sb1("winb", [128, 8, 3584], BF16)
            wob = sb1("wob", [128, 8, D], BF16)
            n1w = sb1("n1w", [128, D], F32)
            n2w = sb1("n2w", [128, D], F32)
            gnw = sb1("gnw", [128, 512], F32)
            rb = sb1("rb", [128, 72], F32)
            cw = sb1("cw", [128, 12], F32)
            mask = sb1("mask", [128, 512], F32)
            wr = sb1("wr_sb", [128, 8, 72], F32)
            tab = [sb1("tab%d" % i, [128, 4, 256], F32) for i in range(2)]
            hb = [sb1("hb%d" % i, [128, D], BF16) for i in range(2)]
            hTg = [sb1("hTg%d" % i, [128, 8, 512], BF16) for i in range(2)]
            qh = [sb1("qh%d" % i, [128, 512], BF16) for i in range(2)]
            kh = [sb1("kh%d" % i, [128, 512], BF16) for i in range(3)]
            vb = [sb1("vb%d" % i, [128, 512], BF16) for i in range(3)]
            gs = [sb1("gs%d" % i, [128, 512], F32) for i in range(3)]
            qT = [sb1("qT%d" % i, [128, 512], BF16) for i in range(2)]
            kT = [sb1("kT%d" % i, [128, 512], BF16) for i in range(2)]
            pT = [sb1("pT%d" % i, [128, 512], BF16) for i in range(2)]
            ra = [sb1("ra%d" % i, [128, 256], F32) for i in range(2)]
            rbt = [sb1("rbt%d" % i, [128, 256], F32) for i in range(2)]
            yn = sb1("yn", [128, 512], F32)
            retb = [sb1("retb%d" % i, [128, 512], BF16) for i in range(2)]
            retT = [sb1("retT%d" % i, [128, 512], BF16) for i in range(2)]
            st6 = sb1("st6", [128, 4, 6], F32)
            mv = sb1("mv", [128, 4, 2], F32)
            rs4 = sb1("rs4", [128, 4], F32)
            csb = sb1("csb", [128, 512], F32)
            cu = sb1("cu", [128, 514], F32)
            halo = sb1("halo", [128, 4, 2], F32)
            t1 = sb1("t1", [128, 512], F32)
            convT = [sb1("convT%d" % i, [128, 4, 512], BF16) for i in range(2)]
            Pst = sb1("Pst", [128, 4, 128], F32)
            Pb = sb1("Pb", [128, 4, 128], BF16)
            xres = [sb1("xres%d" % i, [128, D], F32) for i in range(2)]
            h2f = [sb1("h2f%d" % i, [128, D], F32) for i in range(2)]
            h2bt = [sb1("h2bt%d" % i, [128, D], BF16) for i in range(2)]
            h2T = [sb1("h2T0", [128, 8, 128], F32)] * 2
            rstd2 = sb1("rstd2", [128, 1], F32)
            ssq2 = sb1("ssq2", [128, 1], F32)

            for t_, src in ((n1w, c_n1w), (n2w, c_n2w), (gnw, c_gnw), (rb, c_rb), (cw, c_cw), (mask, c_mask)):
                K.dma("sync", t_[:], src, writes=[t_])
            K.dma("sync", wr[:], wr_d.rearrange("(j p) n -> p j n", p=128), writes=[wr])
            for j in range(8):
                K.dma("pool", winb[:, j, :], w_in[j * 128:(j + 1) * 128, :], writes=[winb], acc=(j > 0))
            K.dma("pool", wob[:], w_o.rearrange("(j p) n -> p j n", p=128), writes=[wob])
            K.memset("dve", Pst[:], 0.0)
            zt = sb1("zt", [128, D], BF16)
            K.memset("pool", zt[:], 0.0)
            K.memset("dve", Pb[:], 0.0)
            K.memset("dve", halo[:], 0.0)

            cnt = {"x": 0, "tr": 0, "pj": 0}

            def tab_load(t):
                tb = tab[t % 2]
                if t < 16:
                    K.dma("sync", tb[:, 0:2, :], tab_pre[t * 128:(t + 1) * 128, :, :], writes=[tb])
                else:
                    K.dma("sync", tb[:], tab_own[(t - 16) * 128:(t - 15) * 128, :, :], writes=[tb])

            def front_load(t):
                x_src = x_pre if t < 16 else x_own
                r0 = (t % 16) * 128
                K.dma("sync", xt[t % 3][:], x_src[r0:r0 + 128, :], writes=[xt[t % 3]])

            def front_sq(t):
                rms_stats(xt[t % 3][:])

            def front_hb(t):
                K.stt(hb[t % 2][:], xt[t % 3][:], rstd[:, 0:1], n1w[:], ALU.mult, ALU.mult)

            def front_stats(t):
                front_sq(t)
                front_hb(t)

            def front_tr(t):
                tix = t % 4
                hT_ = hTg[(t // 4) % 2]
                b = t % 2
                pb = PB[cnt["tr"] % 2]
                cnt["tr"] += 1
                pv = pb[:].rearrange("p (j t) -> p j t", j=8)
                for j in range(8):
                    K.tr(pv[:, j, :], hb[b][:, j * 128:(j + 1) * 128], id16[:])
                K.copy("act", hT_[:, :, tix * 128:(tix + 1) * 128], pv, acc=(tix > 0))

            def front(t):
                front_stats(t)
                front_tr(t)

            def proj_tm(t, col0, pf):
                hT_ = hTg[(t // 4) % 2]
                tix = t % 4
                for j in range(8):
                    K.mm(pf[:], hT_[:, j, tix * 128:(tix + 1) * 128], winb[:, j, col0:col0 + 512], j == 0, j == 7)

            def next_pj():
                pf = PF[cnt["pj"] % 3]
                cnt["pj"] += 1
                return pf

            def rotary(pf, tabt, ci, si, out):
                pv = pf[:].rearrange("p (h two d) -> p h two d", h=4, two=2)
                x1 = pv[:, :, 0, :]
                x2 = pv[:, :, 1, :]
                C = tabt[:, ci, :].rearrange("p (h d) -> p h d", h=4)
                Sn = tabt[:, si, :].rearrange("p (h d) -> p h d", h=4)
                ov = out[:].rearrange("p (h two d) -> p h two d", h=4, two=2)
                a = ra[0][:].rearrange("p (h d) -> p h d", h=4)
                bb = rbt[0][:].rearrange("p (h d) -> p h d", h=4)
                K.tt("dve", a, x1, C, ALU.mult)
                K.tt("dve", bb, x2, Sn, ALU.mult)
                K.tt("pool", ov[:, :, 0, :], a, bb, ALU.subtract)
                a2 = ra[1][:].rearrange("p (h d) -> p h d", h=4)
                bb2 = rbt[1][:].rearrange("p (h d) -> p h d", h=4)
                K.tt("dve", a2, x2, C, ALU.mult)
                K.tt("dve", bb2, x1, Sn, ALU.mult)
                K.tt("pool", ov[:, :, 1, :], a2, bb2, ALU.add, acc=True)

            def kv_update(b, pb_out=True):
                pkv = PF[5]
                pkvv = pkv[:].rearrange("p (h e) -> p h e", h=4)
                for h in range(4):
                    K.mm(pkvv[:, h, :], kh[b][:, h * 128:(h + 1) * 128], vb[b][:, h * 128:(h + 1) * 128], True, True)
                for h in range(4):
                    K.stt(Pst[:, h, :], Pst[:, h, :], cd[h], pkvv[:, h, :], ALU.mult, ALU.add)
                if pb_out:
                    for h in range(4):
                        K.act(Pb[:, h, :], Pst[:, h, :], AF.Copy, scale=cd[h])

            def cu_chunk(Gu, cc):
                hT_ = hTg[Gu % 2]
                pfc, pfu = PF[3], PF[4]
                for which, pf in ((1, pfc), (2, pfu)):
                    c0 = 2048 + which * 512 + cc * 128
                    for j in range(8):
                        K.mm(pf[:], winb[:, j, c0:c0 + 128], hT_[:, j, :], j == 0, j == 7)
                K.copy("act", csb[:], pfc[:])
                K.copy("dve", cu[:, 0:2], halo[:, cc, :])
                K.tt("dve", cu[:, 2:514], csb[:], pfu[:], ALU.mult)
                K.S.add("dve", lambda e, cc=cc: e.tensor_copy(out=halo[:, cc, :], in_=cu[:, 512:514]),
                        reads=[cu], writes=[halo], selfwait=True)

            def prefix_work(t):
                b = t % 3
                tb = tab[t % 2]
                pf = next_pj()
                proj_tm(t, 512, pf)
                rotary(pf, tb, 0, 1, kh[b])
                pf = next_pj()
                proj_tm(t, 1024, pf)
                K.copy("act", vb[b][:], pf[:])

            def conv_group(Gu):
                cvb = convT[Gu % 2]
                hT_ = hTg[Gu % 2]
                for cc in range(4):
                    cu_chunk(Gu, cc)
                    pfb = PF[5]
                    c0 = 2048 + cc * 128
                    for j in range(8):
                        K.mm(pfb[:], winb[:, j, c0:c0 + 128], hT_[:, j, :], j == 0, j == 7)
                    K.ts("dve", t1[:], cu[:, 2:514], cw[:, cc * 3 + 2:cc * 3 + 3], ALU.mult)
                    K.stt(t1[:], cu[:, 1:513], cw[:, cc * 3 + 1:cc * 3 + 2], t1[:], ALU.mult, ALU.add)
                    K.stt(t1[:], cu[:, 0:512], cw[:, cc * 3 + 0:cc * 3 + 1], t1[:], ALU.mult, ALU.add)
                    K.tt("dve", cvb[:, cc, :], t1[:], pfb[:], ALU.mult, acc=(cc > 0))

            def stage_A(t):
                ti = t - 16
                b3 = t % 3
                tb = tab[t % 2]
                pf = next_pj()
                proj_tm(t, 0, pf)
                rotary(pf, tb, 0, 1, qh[t % 2])
                pf = next_pj()
                proj_tm(t, 512, pf)
                rotary(pf, tb, 2, 3, kh[b3])
                pf = next_pj()
                proj_tm(t, 1024, pf)
                K.copy("act", vb[b3][:], pf[:])
                pf = next_pj()
                proj_tm(t, 1536, pf)
                K.act(gs[b3][:], pf[:], AF.Silu)

            def stage_B1(t):
                b = t % 2
                pb = PB[cnt["tr"] % 2]
                cnt["tr"] += 1
                for h in range(4):
                    K.tr(pb[:, h * 128:(h + 1) * 128], qh[b][:, h * 128:(h + 1) * 128], id16[:])
                for h in range(4):
                    K.tr(pb[:, 512 + h * 128:512 + (h + 1) * 128], kh[t % 3][:, h * 128:(h + 1) * 128], id16[:])
                evq = "act" if t % 2 == 0 else "dve"
                K.copy(evq, qT[b][:], pb[:, 0:512])
                K.copy(evq, kT[b][:], pb[:, 512:1024])

            def stage_B2(t):
                b = t % 2
                psc = PF[3]
                for h in range(4):
                    K.mm(psc[:, h * 128:(h + 1) * 128], kT[b][:, h * 128:(h + 1) * 128], qT[b][:, h * 128:(h + 1) * 128], True, True)
                K.tt("dve", pT[b][:], psc[:], mask[:], ALU.mult)

            def stage_B3(t):
                b = t % 2
                b3 = t % 3
                po = PF[4]
                for h in range(4):
                    hs = slice(h * 128, (h + 1) * 128)
                    K.mm(po[:, hs], pT[b][:, hs], vb[b3][:, hs], True, False)
                    K.mm(po[:, hs], qT[b][:, hs], Pb[:, h, :], False, True)
                kv_update(b3)
                for h in range(4):
                    K.S.add("dve", lambda e, h=h: e.bn_stats(out=st6[:, h, :], in_=po[:, h * 128:(h + 1) * 128]),
                            reads=[po], writes=[st6])
                for h in range(4):
                    K.S.add("dve", lambda e, h=h: e.bn_aggr(out=mv[:, h, :], in_=st6[:, h, :]),
                            reads=[st6], writes=[mv])
                rsqrt_pool(rs4[:], mv[:, :, 1], 1.0, 4)
                for h in range(4):
                    hs = slice(h * 128, (h + 1) * 128)
                    K.ts("dve", yn[:, hs], po[:, hs], mv[:, h, 0:1], ALU.subtract, rs4[:, h:h + 1], ALU.mult)
                K.tt("pool", yn[:], yn[:], gnw[:], ALU.mult)
                K.tt("pool", retb[b][:], yn[:], gs[b3][:], ALU.mult)

            def stage_D1(t):
                b = t % 2
                pb = PB[cnt["tr"] % 2]
                cnt["tr"] += 1
                for h in range(4):
                    K.tr(pb[:, h * 128:(h + 1) * 128], retb[b][:, h * 128:(h + 1) * 128], id16[:])
                K.copy("act", retT[b][:], pb[:, 0:512])
                K.dma("sync", xres[b][:], x_own[(t - 16) * 128:(t - 15) * 128, :], writes=[xres[b]])

            def stage_D2(t):
                ti = t - 16
                r0 = ti * 128
                b = t % 2
                cvb = convT[(t // 4) % 2]
                tix = t % 4
                if debug:
                    for nm, tt_ in (("kh", kh[t % 3]), ("vb", vb[t % 3]), ("qh", qh[b]), ("retb", retb[b])):
                        K.dma("sync", dbg[nm][r0:r0 + 128, :], tt_[:], reads=[tt_], writes=["dbg_" + nm], acc=True)
                x1t_ = xres[b]
                for half in range(2):
                    pw = next_pj()
                    for kc in range(8):
                        lhs = retT[b][:, kc * 128:(kc + 1) * 128] if kc < 4 else cvb[:, kc - 4, tix * 128:(tix + 1) * 128]
                        K.mm(pw[:], lhs, wob[:, kc, half * 512:(half + 1) * 512], kc == 0, kc == 7)
                    K.tt("dve", x1t_[:, half * 512:(half + 1) * 512], pw[:], x1t_[:, half * 512:(half + 1) * 512], ALU.add)
                K.dma("act", x1_d[r0:r0 + 128, :], x1t_[:], reads=[x1t_], writes=["x1_d"], acc=True)
                if debug:
                    K.dma("sync", dbg["x1"][r0:r0 + 128, :], x1t_[:], reads=[x1t_], writes=["dbg_x1"], acc=True)
                K.act(junk[:], x1t_[:], AF.Square, accum=ssq2[:])
                rsqrt_pool(rstd2[:], ssq2[:], 1.0 / D, 1)
                K.stt(h2f[b][:], x1t_[:], rstd2[:, 0:1], n2w[:], ALU.mult, ALU.mult)
                K.copy("act", h2bt[b][:], h2f[b][:])
                K.dma("act", h2_d[r0:r0 + 128, :], h2bt[b][:], reads=[h2bt[b]], writes=["h2_d"], acc=True)

            def stage_E1(t, rnd):
                b = t % 2
                p32 = PF[3]
                for jj in range(4):
                    j = rnd * 4 + jj
                    K.tr(p32[:, jj * 128:(jj + 1) * 128], h2f[b][:, j * 128:(j + 1) * 128], id32[:])
                K.copy("act", h2T[b][:, rnd * 4:(rnd + 1) * 4, :], p32[:].rearrange("p (j t) -> p j t", j=4), acc=(rnd > 0))

            def stage_E2(t):
                ti = t - 16
                plog = PF[5]
                for j in range(8):
                    K.mm(plog[:, 0:72], h2T[t % 2][:, j, :], wr[:, j, :], j == 0, j == 7)
                K.tt("dve", LG[:, ti, :], plog[:, 0:72], rb[:], ALU.add)

            own = lambda t: 16 <= t < 32
            for t in range(5):
                front_load(t)
            tab_load(0)
            for t in range(4):
                front(t)
            for s in range(32 + 5):
                nf = s + 4 if s + 4 < 32 else None
                if s + 5 < 32:
                    front_load(s + 5)
                if s + 1 < 32:
                    tab_load(s + 1)
                if nf is not None:
                    front_sq(nf)
                if s < 16:
                    for zi in range(4):
                        blk = s * 4 + zi
                        K.dma("sync", xs_d[blk * 128:(blk + 1) * 128, :], zt[:], reads=[zt], writes=["xs_d"], acc=True)
                    prefix_work(s)
                    if nf is not None:
                        front_hb(nf)
                        front_tr(nf)
                    if s >= 1:
                        kv_update((s - 1) % 3, pb_out=False)
                    if s == 15:
                        kv_update(15 % 3, pb_out=True)
                        for cc in range(4):
                            cu_chunk(3, cc)
                    continue
                if own(s - 2):
                    stage_B2(s - 2)
                if own(s):
                    if s % 4 == 0:
                        conv_group(s // 4)
                    stage_A(s)
                if nf is not None:
                    front_hb(nf)
                if own(s - 3):
                    stage_D1(s - 3)
                if own(s - 2):
                    stage_B3(s - 2)
                if own(s - 4):
                    stage_E1(s - 4, 0)
                if own(s - 1):
                    stage_B1(s - 1)
                if own(s - 4):
                    stage_E1(s - 4, 1)
                if own(s - 3):
                    stage_D2(s - 3)
                if own(s - 4):
                    stage_E2(s - 4)
                if nf is not None:
                    front_tr(nf)
            if debug:
                K.dma("sync", dbg["lg"], LG[:].rearrange("p t n -> p (t n)"), reads=[LG], writes=["dbg_lg"])
            S.emit(p1)

        with ExitStack() as p2:
            if phases < 2:
                return nc
            S = Sched(nc)
            S.all_selfwait = True
            K.S = S
            sb2 = lambda name, shape, dt: sb(name, shape, dt, p2)
            ustr = sb2("ustr", [128, 128], F32)
            ones = sb2("ones", [128, 128], F32)
            base = sb2("base", [128, 64], F32)
            m16 = sb2("m16", [128, NT], F32)
            ohg = sb2("ohg", [128, NT, 8], F32)
            sg = sb2("sg", [128, NT, 8], F32)
            se = sb2("se", [128, NT], F32)
            pg = sb2("pg", [128, NT], F32)
            tmp4 = sb2("tmp4", [128, NT, 8, 8], F32)
            sel = sb2("sel", [128, NT, 8], F32)
            sel2 = sb2("sel2", [128, NT, 8], F32)
            m1 = sb2("m1", [128, NT], F32)
            m2 = sb2("m2", [128, NT], F32)
            oh1 = sb2("oh1", [128, NT, 8], F32)
            oh2 = sb2("oh2", [128, NT, 8], F32)
            dd = sb2("dd", [128, NT], F32)
            OH = [sb2("OH%d" % k, [128, NT, 64], F32) for k in range(2)]
            Osum = sb2("Osum", [128, NT, 64], F32)
            Ccum = sb2("Ccum", [128, NT, 64], F32)
            Rk = sb2("Rk", [128, NT, 64], F32)
            dstf = sb2("dstf", [128, NT], F32)
            hsc = [sb2("hsc%d" % i, [128, D], BF16) for i in range(NT)]
            NB = 4
            wg = [sb2("wg%d" % i, [128, 8, 512], BF16) for i in range(NB)]
            wu = [sb2("wu%d" % i, [128, 8, 512], BF16) for i in range(NB)]
            wd = [sb2("wd%d" % i, [128, 4, D], BF16) for i in range(NB)]
            xb = [sb2("xb%d" % i, [128, D], BF16) for i in range(2)]
            xT = [sb2("xT%d" % i, [128, 8, 128], BF16) for i in range(2)]
            gsl = [sb2("gsl%d" % i, [128, 512], F32) for i in range(2)]
            hT2 = [sb2("hT2%d" % i, [128, 512], BF16) for i in range(2)]
            yb = [sb2("yb%d" % i, [128, D], F32) for i in range(2)]

            def load_w(e_):
                wb_ = e_ % NB
                K.dma("pool", wg[wb_][:], w_gate[e_].rearrange("(j p) n -> p j n", p=128), writes=[wg[wb_]])
                K.dma("pool", wu[wb_][:], w_up[e_].rearrange("(j p) n -> p j n", p=128), writes=[wu[wb_]])
                K.dma("pool", wd[wb_][:], w_down[e_].rearrange("(j p) n -> p j n", p=128), writes=[wd[wb_]])

            if phases >= 3:
                load_w(0)
            for i in range(NT):
                K.dma("sync", hsc[i][:], h2_d[i * 128:(i + 1) * 128, :], reads=["h2_d"], writes=[hsc[i]])
            K.dma("sync", ustr[:], c_ustr, writes=[ustr])
            K.dma("sync", ones[:], c_ones, writes=[ones])
            K.dma("sync", base[:], c_base, writes=[base])
            gl = LG[:, :, 0:8]
            el = LG[:, :, 8:72].rearrange("p t (g e) -> p t g e", g=8)
            bc3 = lambda a: a.unsqueeze(2).to_broadcast([128, NT, 8])
            K.red("dve", m16[:], gl, ALU.max)
            K.tt("dve", ohg[:], gl, bc3(m16[:]), ALU.is_equal)
            K.tt("dve", sg[:], gl, bc3(m16[:]), ALU.subtract)
            K.act(sg[:], sg[:], AF.Exp)
            K.red("dve", se[:], sg[:], ALU.add)
            K.recip(pg[:], se[:])
            K.tt("dve", tmp4[:], el, ohg[:].unsqueeze(3).to_broadcast([128, NT, 8, 8]), ALU.mult)
            K.red("dve", sel[:], tmp4[:].rearrange("p t g e -> p t e g"), ALU.add)
            K.red("dve", m1[:], sel[:], ALU.max)
            K.tt("dve", oh1[:], sel[:], bc3(m1[:]), ALU.is_equal)
            K.stt(sel2[:], oh1[:], NEG, sel[:], ALU.mult, ALU.add)
            K.red("dve", m2[:], sel2[:], ALU.max)
            K.tt("dve", oh2[:], sel2[:], bc3(m2[:]), ALU.is_equal)
            K.tt("dve", dd[:], m2[:], m1[:], ALU.subtract)
            K.act(dd[:], dd[:], AF.Exp)
            K.ts("dve", dd[:], dd[:], 1.0, ALU.add)
            K.recip(dd[:], dd[:])
            K.tt("dve", gw[0][:], pg[:], dd[:], ALU.mult)
            K.tt("dve", gw[1][:], pg[:], gw[0][:], ALU.subtract)
            for k, oh in ((0, oh1), (1, oh2)):
                K.tt("dve", OH[k][:].rearrange("p t (g e) -> p t g e", g=8),
                     ohg[:].unsqueeze(3).to_broadcast([128, NT, 8, 8]),
                     oh[:].unsqueeze(2).to_broadcast([128, NT, 8, 8]), ALU.mult)
            K.tt("dve", Osum[:], OH[0][:], OH[1][:], ALU.add)
            K.memset("dve", Ccum[:, 0, :], 0.0)
            for i in range(1, NT):
                K.tt("dve", Ccum[:, i, :], Ccum[:, i - 1, :], Osum[:, i - 1, :], ALU.add)
            for i in range(NT):
                pr = PF[i // 8]
                o0 = (i % 8) * 64
                K.mm(pr[:, o0:o0 + 64], ustr[:], Osum[:, i, :], True, False)
                K.mm(pr[:, o0:o0 + 64], ones[:], Ccum[:, i, :], False, True)
            for hf in range(2):
                K.tt("dve", Rk[:, hf * 8:(hf + 1) * 8, :], PF[hf][:].rearrange("p (t n) -> p t n", t=8),
                     base[:].unsqueeze(1).to_broadcast([128, 8, 64]), ALU.add)
            for k in range(2):
                K.tt("dve", OH[k][:], OH[k][:], Rk[:], ALU.mult)
                K.red("dve", dstf[:], OH[k][:], ALU.add)
                K.copy("dve", dst[k][:], dstf[:])
            if debug:
                rt = sb2("rtdbg", [128, 4 * NT], F32)
                K.copy("dve", rt[:, 0:NT], dst[0][:])
                K.copy("dve", rt[:, NT:2 * NT], dst[1][:])
                K.copy("dve", rt[:, 2 * NT:3 * NT], gw[0][:])
                K.copy("dve", rt[:, 3 * NT:4 * NT], gw[1][:])
                K.dma("sync", dbg["rt"], rt[:], reads=[rt], writes=["dbg_rt"])
            for i in range(NT):
                hb_ = hsc[i]
                for k in range(2):
                    K.S.add("pool", lambda e, i=i, k=k, hb_=hb_: e.indirect_dma_start(
                        out=xs_d, out_offset=bass.IndirectOffsetOnAxis(ap=dst[k][:, i:i + 1], axis=0),
                        in_=hb_[:], in_offset=None, bounds_check=bnd(e), oob_is_err=False),
                        reads=[hb_, dst[k]], writes=["xs_d"], dma=True, acc=True)
            if phases >= 3:
                for e_ in range(1, NB):
                    load_w(e_)
                for e_ in range(NE):
                    wb_ = e_ % NB
                    b = e_ % 2
                    if e_ >= NB:
                        load_w(e_)
                    K.dma("sync", xb[b][:], xs_d[e_ * 128:(e_ + 1) * 128, :], reads=["xs_d"], writes=[xb[b]])
                    pb = PB[e_ % 2]
                    pv = pb[:].rearrange("p (j t) -> p j t", j=8)
                    for j in range(8):
                        K.tr(pv[:, j, :], xb[b][:, j * 128:(j + 1) * 128], id16[:])
                    K.copy("act" if e_ % 2 == 0 else "dve", xT[b][:], pv)
                    pgt = PF[0 + 2 * b]
                    put = PF[1 + 2 * b]
                    for w_, pf in ((wg[wb_], pgt), (wu[wb_], put)):
                        for f in range(4):
                            for j in range(8):
                                K.mm(pf[:, f * 128:(f + 1) * 128], w_[:, j, f * 128:(f + 1) * 128], xT[b][:, j, :], j == 0, j == 7)
                    K.act(gsl[b][:], pgt[:], AF.Silu)
                    K.tt("dve", hT2[b][:], gsl[b][:], put[:], ALU.mult)
                    for half in range(2):
                        py = PF[4 + half]
                        for f in range(4):
                            K.mm(py[:], hT2[b][:, f * 128:(f + 1) * 128], wd[wb_][:, f, half * 512:(half + 1) * 512], f == 0, f == 3)
                    K.copy("act", yb[b][:, 0:512], PF[4][:])
                    K.copy("dve", yb[b][:, 512:1024], PF[5][:], acc=True)
                    K.dma("act", ys_d[e_ * 128:(e_ + 1) * 128, :], yb[b][:], reads=[yb[b]], writes=["ys_d"], acc=True)

            S.emit(p2)


        with ExitStack() as p4:
            if phases < 4:
                return nc
            S = Sched(nc)
            K.S = S
            sb4 = lambda name, shape, dt: sb(name, shape, dt, p4)
            fnw = sb4("fnw", [128, D], F32)
            NB4 = 4
            y0 = [sb4("y0_%d" % i, [128, D], F32) for i in range(NB4)]
            y1 = [sb4("y1_%d" % i, [128, D], F32) for i in range(NB4)]
            xo = [sb4("xo%d" % i, [128, D], F32) for i in range(NB4)]
            ot = [sb4("ot%d" % i, [128, D], F32) for i in range(2)]
            ssq4 = [sb4("ssq4_%d" % i, [128, 1], F32) for i in range(NB4)]
            rst4 = [sb4("rst4_%d" % i, [128, 1], F32) for i in range(NB4)]
            K.dma("sync", fnw[:], c_fnw, writes=[fnw])

            def p4_load(i):
                b = i % NB4
                r0 = i * 128
                K.dma("sync", xo[b][:], x1_d[r0:r0 + 128, :], reads=["x1_d"], writes=[xo[b]])
                for k, yk in ((0, y0[b]), (1, y1[b])):
                    K.S.add("pool", lambda e, i=i, k=k, yk=yk: e.indirect_dma_start(
                        out=yk[:], out_offset=None, in_=ys_d,
                        in_offset=bass.IndirectOffsetOnAxis(ap=dst[k][:, i:i + 1], axis=0),
                        bounds_check=bnd(e), oob_is_err=False),
                        reads=["ys_d", dst[k]], writes=[yk], dma=True)

            def p4_mid(i):
                b = i % NB4
                K.stt(xo[b][:], y0[b][:], gw[0][:, i:i + 1], xo[b][:], ALU.mult, ALU.add)
                K.stt(xo[b][:], y1[b][:], gw[1][:, i:i + 1], xo[b][:], ALU.mult, ALU.add)
                K.act(junk[:], xo[b][:], AF.Square, accum=ssq4[b][:])
                rsqrt_pool(rst4[b][:], ssq4[b][:], 1.0 / D, 1)

            def p4_fin(i):
                b = i % NB4
                r0 = i * 128
                K.stt(ot[i % 2][:], xo[b][:], rst4[b][:, 0:1], fnw[:], ALU.mult, ALU.mult)
                K.dma("act", y_out[r0:r0 + 128, :], ot[i % 2][:], reads=[ot[i % 2]], writes=["y_out"], acc=True)

            p4_load(0)
            p4_load(1)
            for s_ in range(NT + 1):
                if s_ + 2 < NT:
                    p4_load(s_ + 2)
                if s_ < NT:
                    p4_mid(s_)
                if 0 <= s_ - 1 < NT:
                    p4_fin(s_ - 1)
            S.emit(p4)
    return nc


def _tables():
    half = 64
    inv = (10000.0 ** (-np.arange(half, dtype=np.float32) / half)).astype(np.float32)
    pos = np.arange(SEQ, dtype=np.float32)
    ang = (pos[:, None] * inv[None, :]).astype(np.float32).astype(np.float64)
    cos = np.cos(ang)
    sin = np.sin(ang)
    logg = np.log1p(-(2.0 ** (-5.0 - np.arange(4, dtype=np.float64))))
    i = (np.arange(SEQ) % 128).astype(np.float64)
    dq = np.exp(logg[None, :] * (i[:, None] + 1.0 - 128.0)) * (128.0 ** -0.5)
    dk = np.exp(logg[None, :] * (127.0 - i[:, None]))
    tq_c = dq[:, :, None] * cos[:, None, :]
    tq_s = dq[:, :, None] * sin[:, None, :]
    tk_c = dk[:, :, None] * cos[:, None, :]
    tk_s = dk[:, :, None] * sin[:, None, :]
    full = np.stack([tq_c, tq_s, tk_c, tk_s], axis=1).reshape(SEQ, 4, 256).astype(np.float32)
    return full


def _prep(inputs):
    f = lambda a: np.ascontiguousarray(np.asarray(a, dtype=np.float32))
    x = f(inputs["x"])
    full = _tables()
    rep = lambda v: np.ascontiguousarray(np.broadcast_to(f(v).reshape(1, -1), (128, f(v).size)))
    wr = np.concatenate([f(inputs["router_g_w"])[0]] + [f(inputs["router_e_w"])[0, g] for g in range(8)], axis=1)
    rbias = np.concatenate([f(inputs["router_g_b"])[0].reshape(-1), f(inputs["router_e_b"])[0].reshape(-1)])
    cwv = f(inputs["conv_w"])[0]
    cw = np.ascontiguousarray(cwv.reshape(3, 4, 128).transpose(2, 1, 0).reshape(128, 12))
    jj, ii = np.meshgrid(np.arange(128), np.arange(128), indexing="ij")
    maskT = (ii >= jj).astype(np.float32)
    common = {
        "w_in": f(inputs["w_in"])[0], "w_o": f(inputs["w_o"])[0],
        "w_gate": f(inputs["w_gate"])[0], "w_up": f(inputs["w_up"])[0], "w_down": f(inputs["w_down"])[0],
        "wr": np.ascontiguousarray(wr),
        "c_n1w": rep(inputs["norm1_w"]), "c_n2w": rep(inputs["norm2_w"]), "c_fnw": rep(inputs["final_norm_w"]),
        "c_gnw": rep(inputs["ret_gn_w"]), "c_rb": rep(rbias), "c_cw": cw,
        "c_id16": np.eye(128).astype(ml_dtypes.bfloat16), "c_id32": np.eye(128, dtype=np.float32),
        "c_mask": np.ascontiguousarray(np.tile(maskT, (1, 4))),
        "c_ustr": (jj < ii).astype(np.float32),
        "c_ones": np.ones((128, 128), np.float32),
        "c_base": np.ascontiguousarray(np.broadcast_to((np.arange(64, dtype=np.float32) * 128.0)[None, :], (128, 64))),
    }
    in_maps = []
    for c in range(NCORES):
        b, hf = c // 2, c % 2
        m = dict(common)
        m["x_own"] = np.ascontiguousarray(x[b, hf * TOK:(hf + 1) * TOK])
        m["x_pre"] = np.ascontiguousarray(x[b, 0:TOK]) if hf == 1 else np.zeros((TOK, D), np.float32)
        m["tab_own"] = np.ascontiguousarray(full[hf * TOK:(hf + 1) * TOK])
        m["tab_pre"] = np.ascontiguousarray(full[0:TOK, 2:4])
        in_maps.append(m)
    return in_maps


_NC_CACHE = {}


def kernel(**inputs):
    in_maps = _prep(inputs)
    if "nc" not in _NC_CACHE:
        _NC_CACHE["nc"] = build_nc(0)
    res = run_bass_kernel_spmd(_NC_CACHE["nc"], in_maps, core_ids=list(range(NCORES)))
    out = np.empty((4, SEQ, D), np.float32)
    for c in range(NCORES):
        b, hf = c // 2, c % 2
        out[b, hf * TOK:(hf + 1) * TOK] = res.results[c]["y_out"]
    return out
```

```python
import numpy as np
import ml_dtypes
from contextlib import ExitStack
import concourse.bass as bass
import concourse.mybir as mybir
from concourse.bass_utils import run_bass_kernel_spmd

F32 = mybir.dt.float32
BF16 = mybir.dt.bfloat16
I32 = mybir.dt.int32
AF = mybir.ActivationFunctionType
ALU = mybir.AluOpType
AX = mybir.AxisListType

NCORES = 8
D = 1024
SEQ = 4096
TOK = 2048
NT = TOK // 128
NE = 64
EPS = 1e-6
NEG = -1.0e30

ENGS = ("sync", "act", "pool", "pe", "dve")
NDMASEM = 8


class Op:
    __slots__ = ("eng", "fn", "deps", "dma", "needs_inc", "seq", "sem", "semval", "guard", "selfwait", "raw")

    def __init__(self, eng, fn, deps, dma):
        self.eng = eng
        self.fn = fn
        self.deps = deps
        self.dma = dma
        self.needs_inc = False
        self.seq = 0
        self.sem = None
        self.semval = 0
        self.guard = None
        self.selfwait = False


class Sched:
    def __init__(self, nc):
        self.nc = nc
        self.ops = {e: [] for e in ENGS}
        self.res = {}
        self.dma_hist = {e: [] for e in ENGS}
        self.uid = 0
        self.all_selfwait = True

    @staticmethod
    def _name(x):
        if isinstance(x, str):
            return x
        if hasattr(x, "tensor"):
            return x.tensor.name
        return x.name

    def add(self, eng, fn, reads=(), writes=(), dma=False, acc=False, selfwait=False):
        deps = {}
        raw = set()
        for r in reads:
            st = self.res.get(self._name(r))
            if st is not None:
                for o in st[0].values():
                    deps[id(o)] = o
                    raw.add(id(o))
        for w in writes:
            st = self.res.get(self._name(w))
            if st is not None:
                for o in st[1].values():
                    deps[id(o)] = o
                if not acc:
                    for o in st[0].values():
                        deps[id(o)] = o
                        raw.add(id(o))
                else:
                    for o in st[2].values():
                        deps[id(o)] = o
        op = Op(eng, fn, list(deps.values()), dma)
        op.selfwait = (selfwait or self.all_selfwait) and eng != "pe"
        op.raw = raw
        if dma:
            hist = self.dma_hist[eng]
            i = len(hist)
            if i >= NDMASEM:
                op.guard = hist[i - NDMASEM]
            op.sem = i % NDMASEM
            op.semval = 16 * (i // NDMASEM + 1)
            hist.append(op)
        self.uid += 1
        key = ("dma", self.uid) if dma else eng
        for r in reads:
            st = self.res.setdefault(self._name(r), ({}, {}, {}))
            st[1][key] = op
        for w in writes:
            st = self.res.setdefault(self._name(w), ({}, {}, {}))
            if not acc:
                st[2].clear()
                for k_, o in st[0].items():
                    st[2][("w", k_)] = o
                for k_, o in st[1].items():
                    st[2][("r", k_)] = o
                st[0].clear()
                st[1].clear()
            st[0][key] = op
        self.ops[eng].append(op)
        return op

    def barrier_all(self):
        last = []
        for e in ENGS:
            for o in reversed(self.ops[e]):
                if o.fn is not None and not o.dma:
                    last.append(o)
                    break
            for o in self.dma_hist[e][-NDMASEM:]:
                last.append(o)
        for e in ENGS:
            self.ops[e].append(Op(e, None, list(last), False))
        self.res.clear()

    def emit(self, stack):
        nc = self.nc
        for e in ENGS:
            for op in self.ops[e]:
                for d in op.deps:
                    if d.dma or d.eng != e or (op.selfwait and id(d) in op.raw):
                        d.needs_inc = True
        esem = {e: stack.enter_context(nc.semaphore("s_" + e)) for e in ENGS}
        dsem = {e: [stack.enter_context(nc.semaphore("d_%s%d" % (e, i))) for i in range(NDMASEM)]
                for e in ("sync", "act", "pool")}
        for e in ENGS:
            n = 0
            for op in self.ops[e]:
                if op.dma:
                    continue
                if op.needs_inc:
                    n += 1
                    op.seq = n
        block = stack.enter_context(nc.Block())
        hw = {"sync": block.sync, "act": block.scalar, "pool": block.gpsimd,
              "pe": block.tensor, "dve": block.vector}

        def make(e):
            def body(eng):
                waited = {}

                def wait(sem, key, val):
                    if waited.get(key, 0) >= val:
                        return
                    waited[key] = val
                    eng.wait_ge(sem, val)

                for op in self.ops[e]:
                    for d in op.deps:
                        if d.dma:
                            wait(dsem[d.eng][d.sem], ("d", d.eng, d.sem), d.semval)
                        elif d.eng != e or (op.selfwait and id(d) in op.raw):
                            wait(esem[d.eng], ("e", d.eng), d.seq)
                    if op.guard is not None:
                        g = op.guard
                        wait(dsem[e][g.sem], ("d", e, g.sem), g.semval)
                    if op.fn is None:
                        continue
                    ins = op.fn(eng)
                    if op.dma:
                        ins.then_inc(dsem[e][op.sem], 16)
                    elif op.needs_inc:
                        ins.then_inc(esem[e], 1)
                for o in self.dma_hist[e][-NDMASEM:]:
                    wait(dsem[e][o.sem], ("d", e, o.sem), o.semval)
            return body

        for e in ENGS:
            hw[e](make(e))


class KB:
    def __init__(self, nc, S):
        self.nc = nc
        self.S = S

    def dma(self, q, out, in_, reads=(), writes=(), acc=False):
        return self.S.add(q, lambda e: e.dma_start(out=out, in_=in_), reads=reads, writes=writes, dma=True, acc=acc)

    def mm(self, out, lhsT, rhs, start, stop):
        return self.S.add("pe", lambda e: e.matmul(out, lhsT=lhsT, rhs=rhs, start=start, stop=stop),
                          reads=[lhsT, rhs], writes=[out])

    def tr(self, out, in_, ident):
        return self.S.add("pe", lambda e: e.transpose(out=out, in_=in_, identity=ident),
                          reads=[in_, ident], writes=[out])

    def act(self, out, in_, func, bias=None, scale=None, accum=None, eng="act", acc=False, extra_w=()):
        kw = {}
        rd = [in_]
        if bias is not None:
            kw["bias"] = bias
            if not isinstance(bias, (int, float)):
                rd.append(bias)
        if scale is not None:
            kw["scale"] = scale
            if not isinstance(scale, (int, float)):
                rd.append(scale)
        wr = [out]
        if accum is not None:
            kw["accum_out"] = accum
            wr.append(accum)
        return self.S.add(eng, lambda e: e.activation(out=out, in_=in_, func=func, **kw), reads=rd, writes=wr, acc=acc)

    def tt(self, eng, out, in0, in1, op, acc=False):
        return self.S.add(eng, lambda e: e.tensor_tensor(out=out, in0=in0, in1=in1, op=op),
                          reads=[in0, in1], writes=[out], acc=acc)

    def ts(self, eng, out, in0, s1, op0, s2=None, op1=None, acc=False):
        rd = [in0]
        for s in (s1, s2):
            if s is not None and not isinstance(s, (int, float)):
                rd.append(s)
        if op1 is None:
            fn = lambda e: e.tensor_scalar(out=out, in0=in0, scalar1=s1, scalar2=None, op0=op0)
        else:
            fn = lambda e: e.tensor_scalar(out=out, in0=in0, scalar1=s1, scalar2=s2, op0=op0, op1=op1)
        return self.S.add(eng, fn, reads=rd, writes=[out], acc=acc)

    def stt(self, out, in0, scalar, in1, op0, op1, acc=False):
        rd = [in0, in1]
        if not isinstance(scalar, (int, float)):
            rd.append(scalar)
        return self.S.add("dve", lambda e: e.scalar_tensor_tensor(out=out, in0=in0, scalar=scalar, in1=in1, op0=op0, op1=op1),
                          reads=rd, writes=[out], acc=acc)

    def copy(self, eng, out, in_, acc=False):
        if eng == "act":
            return self.S.add(eng, lambda e: e.copy(out=out, in_=in_), reads=[in_], writes=[out], acc=acc)
        return self.S.add(eng, lambda e: e.tensor_copy(out=out, in_=in_), reads=[in_], writes=[out], acc=acc)

    def red(self, eng, out, in_, op, acc=False):
        return self.S.add(eng, lambda e: e.tensor_reduce(out=out, in_=in_, axis=AX.X, op=op),
                          reads=[in_], writes=[out], acc=acc)

    def recip(self, out, in_):
        return self.S.add("dve", lambda e: e.reciprocal(out=out, in_=in_), reads=[in_], writes=[out])

    def memset(self, eng, ap, val):
        return self.S.add(eng, lambda e: e.memset(ap, val), writes=[ap])


def build_nc(debug=0, phases=4):
    nc = bass.Bass("TRN2", target_bir_lowering=False)
    din = lambda n, s, dt=F32: nc.dram_tensor(n, s, dt, kind="ExternalInput").ap()
    x_own = din("x_own", [TOK, D])
    x_pre = din("x_pre", [TOK, D])
    tab_own = din("tab_own", [TOK, 4, 256])
    tab_pre = din("tab_pre", [TOK, 2, 256])
    w_in = din("w_in", [D, 3584])
    w_o = din("w_o", [D, D])
    if phases >= 3:
        w_gate = din("w_gate", [NE, D, 512])
        w_up = din("w_up", [NE, D, 512])
        w_down = din("w_down", [NE, 512, D])
    wr_d = din("wr", [D, 72])
    c_n1w = din("c_n1w", [128, D])
    c_n2w = din("c_n2w", [128, D])
    c_fnw = din("c_fnw", [128, D])
    c_gnw = din("c_gnw", [128, 512])
    c_rb = din("c_rb", [128, 72])
    c_cw = din("c_cw", [128, 12])
    c_id16 = din("c_id16", [128, 128], BF16)
    c_id32 = din("c_id32", [128, 128])
    c_mask = din("c_mask", [128, 512])
    c_ustr = din("c_ustr", [128, 128])
    c_ones = din("c_ones", [128, 128])
    c_base = din("c_base", [128, 64])
    y_out = nc.dram_tensor("y_out", [TOK, D], F32, kind="ExternalOutput").ap()
    dscr = lambda n, s, dt: nc.dram_tensor(n, s, dt, kind="Internal").ap()
    xs_d = dscr("xs_d", [NE * 128, D], BF16)
    ys_d = dscr("ys_d", [NE * 128, D], F32)
    x1_d = dscr("x1_d", [TOK, D], F32)
    h2_d = dscr("h2_d", [TOK, D], BF16)
    dbg = {}
    if debug:
        dbg["lg"] = nc.dram_tensor("dbg_lg", [128, NT * 72], F32, kind="ExternalOutput").ap()
        dbg["x1"] = nc.dram_tensor("dbg_x1", [TOK, D], F32, kind="ExternalOutput").ap()
        dbg["rt"] = nc.dram_tensor("dbg_rt", [128, 4 * NT], F32, kind="ExternalOutput").ap()
        for nm in ("kh", "vb", "qh", "retb"):
            dbg[nm] = nc.dram_tensor("dbg_" + nm, [TOK, 512], BF16, kind="ExternalOutput").ap()

    REG = {}

    def bnd(e):
        if "r" not in REG:
            REG["r"] = e.alloc_register("bnd")
            e.reg_mov(REG["r"], NE * 128 - 1)
        return REG["r"]

    cd = [float(np.exp(np.log1p(-(2.0 ** (-5.0 - h))) * 128.0)) for h in range(4)]

    with ExitStack() as st:
        S = Sched(nc)
        K = KB(nc, S)
        sb = lambda name, shape, dt, stk=st: stk.enter_context(nc.sbuf_tensor(name, shape, dt))
        ps = lambda name, shape, dt: st.enter_context(nc.psum_tensor(name, shape, dt))
        PB = [ps("PB%d" % i, [128, 1024], BF16) for i in range(2)]
        PF = [ps("PF%d" % i, [128, 512], F32) for i in range(6)]
        id16 = sb("id16", [128, 128], BF16)
        id32 = sb("id32", [128, 128], F32)
        epsb = sb("epsb", [128, 1], F32)
        LG = sb("LG", [128, NT, 72], F32)
        dst = [sb("dst%d" % k, [128, NT], I32) for k in range(2)]
        gw = [sb("gw%d" % k, [128, NT], F32) for k in range(2)]
        xt = [sb("xt%d" % i, [128, D], F32) for i in range(3)]
        junk = sb("junk", [128, D], BF16)
        ssq = sb("ssq", [128, 1], F32)
        rstd = sb("rstd", [128, 1], F32)

        K.dma("sync", id16[:], c_id16, writes=[id16])
        K.dma("sync", id32[:], c_id32, writes=[id32])
        K.memset("dve", epsb[:], EPS)
        negh = sb("negh", [128, 8], F32)
        K.memset("pool", negh[:], -0.5)

        def rsqrt_pool(out, in_, scale, n):
            K.ts("pool", out, in_, scale, ALU.mult, EPS, ALU.add)
            K.tt("pool", out, out, negh[:, 0:n], ALU.pow)

        def rms_stats(src, n=D):
            K.act(junk[:, 0:n], src, AF.Square, accum=ssq[:])
            rsqrt_pool(rstd[:], ssq[:], 1.0 / n, 1)

        with ExitStack() as p1:
            sb1 = lambda name, shape, dt: sb(name, shape, dt, p1)
            winb = sb1("winb", [128, 8, 3584], BF16)
            wob = sb1("wob", [128, 8, D], BF16)
            n1w = sb1("n1w", [128, D], F32)
            n2w = sb1("n2w", [128, D], F32)
            gnw = sb1("gnw", [128, 512], F32)
            rb = sb1("rb", [128, 72], F32)
            cw = sb1("cw", [128, 12], F32)
            mask = sb1("mask", [128, 512], F32)
            wr = sb1("wr_sb", [128, 8, 72], F32)
            tab = [sb1("tab%d" % i, [128, 4, 256], F32) for i in range(2)]
            hb = [sb1("hb%d" % i, [128, D], BF16) for i in range(2)]
            hTg = [sb1("hTg%d" % i, [128, 8, 512], BF16) for i in range(2)]
            qh = [sb1("qh%d" % i, [128, 512], BF16) for i in range(2)]
            kh = [sb1("kh%d" % i, [128, 512], BF16) for i in range(3)]
            vb = [sb1("vb%d" % i, [128, 512], BF16) for i in range(3)]
            gs = [sb1("gs%d" % i, [128, 512], F32) for i in range(3)]
            qT = [sb1("qT%d" % i, [128, 512], BF16) for i in range(2)]
            kT = [sb1("kT%d" % i, [128, 512], BF16) for i in range(2)]
            pT = [sb1("pT%d" % i, [128, 512], BF16) for i in range(2)]
            ra = [sb1("ra%d" % i, [128, 256], F32) for i in range(2)]
            rbt = [sb1("rbt%d" % i, [128, 256], F32) for i in range(2)]
            yn = sb1("yn", [128, 512], F32)
            retb = [sb1("retb%d" % i, [128, 512], BF16) for i in range(2)]
            retT = [sb1("retT%d" % i, [128, 512], BF16) for i in range(2)]
            st6 = sb1("st6", [128, 4, 6], F32)
            mv = sb1("mv", [128, 4, 2], F32)
            rs4 = sb1("rs4", [128, 4], F32)
            csb = sb1("csb", [128, 512], F32)
            cu = sb1("cu", [128, 514], F32)
            halo = sb1("halo", [128, 4, 2], F32)
            t1 = sb1("t1", [128, 512], F32)
            convT = [sb1("convT%d" % i, [128, 4, 512], BF16) for i in range(2)]
            Pst = sb1("Pst", [128, 4, 128], F32)
            Pb = sb1("Pb", [128, 4, 128], BF16)
            xres = [sb1("xres%d" % i, [128, D], F32) for i in range(2)]
            h2f = [sb1("h2f%d" % i, [128, D], F32) for i in range(2)]
            h2bt = [sb1("h2bt%d" % i, [128, D], BF16) for i in range(2)]
            h2T = [sb1("h2T0", [128, 8, 128], F32)] * 2
            rstd2 = sb1("rstd2", [128, 1], F32)
            ssq2 = sb1("ssq2", [128, 1], F32)

            for t_, src in ((n1w, c_n1w), (n2w, c_n2w), (gnw, c_gnw), (rb, c_rb), (cw, c_cw), (mask, c_mask)):
                K.dma("sync", t_[:], src, writes=[t_])
            K.dma("sync", wr[:], wr_d.rearrange("(j p) n -> p j n", p=128), writes=[wr])
            for j in range(8):
                K.dma("pool", winb[:, j, :], w_in[j * 128:(j + 1) * 128, :], writes=[winb], acc=(j > 0))
            K.dma("pool", wob[:], w_o.rearrange("(j p) n -> p j n", p=128), writes=[wob])
            K.memset("dve", Pst[:], 0.0)
            zt = sb1("zt", [128, D], BF16)
            K.memset("pool", zt[:], 0.0)
            K.memset("dve", Pb[:], 0.0)
            K.memset("dve", halo[:], 0.0)

            cnt = {"x": 0, "tr": 0, "pj": 0}

            def tab_load(t):
                tb = tab[t % 2]
                if t < 16:
                    K.dma("sync", tb[:, 0:2, :], tab_pre[t * 128:(t + 1) * 128, :, :], writes=[tb])
                else:
                    K.dma("sync", tb[:], tab_own[(t - 16) * 128:(t - 15) * 128, :, :], writes=[tb])

            def front_load(t):
                x_src = x_pre if t < 16 else x_own
                r0 = (t % 16) * 128
                K.dma("sync", xt[t % 3][:], x_src[r0:r0 + 128, :], writes=[xt[t % 3]])

            def front_sq(t):
                rms_stats(xt[t % 3][:])

            def front_hb(t):
                K.stt(hb[t % 2][:], xt[t % 3][:], rstd[:, 0:1], n1w[:], ALU.mult, ALU.mult)

            def front_stats(t):
                front_sq(t)
                front_hb(t)

            def front_tr(t):
                tix = t % 4
                hT_ = hTg[(t // 4) % 2]
                b = t % 2
                pb = PB[cnt["tr"] % 2]
                cnt["tr"] += 1
                pv = pb[:].rearrange("p (j t) -> p j t", j=8)
                for j in range(8):
                    K.tr(pv[:, j, :], hb[b][:, j * 128:(j + 1) * 128], id16[:])
                K.copy("act", hT_[:, :, tix * 128:(tix + 1) * 128], pv, acc=(tix > 0))

            def front(t):
                front_stats(t)
                front_tr(t)

            def proj_tm(t, col0, pf):
                hT_ = hTg[(t // 4) % 2]
                tix = t % 4
                for j in range(8):
                    K.mm(pf[:], hT_[:, j, tix * 128:(tix + 1) * 128], winb[:, j, col0:col0 + 512], j == 0, j == 7)

            def next_pj():
                pf = PF[cnt["pj"] % 3]
                cnt["pj"] += 1
                return pf

            def rotary(pf, tabt, ci, si, out):
                pv = pf[:].rearrange("p (h two d) -> p h two d", h=4, two=2)
                x1 = pv[:, :, 0, :]
                x2 = pv[:, :, 1, :]
                C = tabt[:, ci, :].rearrange("p (h d) -> p h d", h=4)
                Sn = tabt[:, si, :].rearrange("p (h d) -> p h d", h=4)
                ov = out[:].rearrange("p (h two d) -> p h two d", h=4, two=2)
                a = ra[0][:].rearrange("p (h d) -> p h d", h=4)
                bb = rbt[0][:].rearrange("p (h d) -> p h d", h=4)
                K.tt("dve", a, x1, C, ALU.mult)
                K.tt("dve", bb, x2, Sn, ALU.mult)
                K.tt("pool", ov[:, :, 0, :], a, bb, ALU.subtract)
                a2 = ra[1][:].rearrange("p (h d) -> p h d", h=4)
                bb2 = rbt[1][:].rearrange("p (h d) -> p h d", h=4)
                K.tt("dve", a2, x2, C, ALU.mult)
                K.tt("dve", bb2, x1, Sn, ALU.mult)
                K.tt("pool", ov[:, :, 1, :], a2, bb2, ALU.add, acc=True)

            def kv_update(b, pb_out=True):
                pkv = PF[5]
                pkvv = pkv[:].rearrange("p (h e) -> p h e", h=4)
                for h in range(4):
                    K.mm(pkvv[:, h, :], kh[b][:, h * 128:(h + 1) * 128], vb[b][:, h * 128:(h + 1) * 128], True, True)
                for h in range(4):
                    K.stt(Pst[:, h, :], Pst[:, h, :], cd[h], pkvv[:, h, :], ALU.mult, ALU.add)
                if pb_out:
                    for h in range(4):
                        K.act(Pb[:, h, :], Pst[:, h, :], AF.Copy, scale=cd[h])

            def cu_chunk(Gu, cc):
                hT_ = hTg[Gu % 2]
                pfc, pfu = PF[3], PF[4]
                for which, pf in ((1, pfc), (2, pfu)):
                    c0 = 2048 + which * 512 + cc * 128
                    for j in range(8):
                        K.mm(pf[:], winb[:, j, c0:c0 + 128], hT_[:, j, :], j == 0, j == 7)
                K.copy("act", csb[:], pfc[:])
                K.copy("dve", cu[:, 0:2], halo[:, cc, :])
                K.tt("dve", cu[:, 2:514], csb[:], pfu[:], ALU.mult)
                K.S.add("dve", lambda e, cc=cc: e.tensor_copy(out=halo[:, cc, :], in_=cu[:, 512:514]),
                        reads=[cu], writes=[halo], selfwait=True)

            def prefix_work(t):
                b = t % 3
                tb = tab[t % 2]
                pf = next_pj()
                proj_tm(t, 512, pf)
                rotary(pf, tb, 0, 1, kh[b])
                pf = next_pj()
                proj_tm(t, 1024, pf)
                K.copy("act", vb[b][:], pf[:])

            def conv_group(Gu):
                cvb = convT[Gu % 2]
                hT_ = hTg[Gu % 2]
                for cc in range(4):
                    cu_chunk(Gu, cc)
                    pfb = PF[5]
                    c0 = 2048 + cc * 128
                    for j in range(8):
                        K.mm(pfb[:], winb[:, j, c0:c0 + 128], hT_[:, j, :], j == 0, j == 7)
                    K.ts("dve", t1[:], cu[:, 2:514], cw[:, cc * 3 + 2:cc * 3 + 3], ALU.mult)
                    K.stt(t1[:], cu[:, 1:513], cw[:, cc * 3 + 1:cc * 3 + 2], t1[:], ALU.mult, ALU.add)
                    K.stt(t1[:], cu[:, 0:512], cw[:, cc * 3 + 0:cc * 3 + 1], t1[:], ALU.mult, ALU.add)
                    K.tt("dve", cvb[:, cc, :], t1[:], pfb[:], ALU.mult, acc=(cc > 0))

            def stage_A(t):
                ti = t - 16
                b3 = t % 3
                tb = tab[t % 2]
                pf = next_pj()
                proj_tm(t, 0, pf)
                rotary(pf, tb, 0, 1, qh[t % 2])
                pf = next_pj()
                proj_tm(t, 512, pf)
                rotary(pf, tb, 2, 3, kh[b3])
                pf = next_pj()
                proj_tm(t, 1024, pf)
                K.copy("act", vb[b3][:], pf[:])
                pf = next_pj()
                proj_tm(t, 1536, pf)
                K.act(gs[b3][:], pf[:], AF.Silu)

            def stage_B1(t):
                b = t % 2
                pb = PB[cnt["tr"] % 2]
                cnt["tr"] += 1
                for h in range(4):
                    K.tr(pb[:, h * 128:(h + 1) * 128], qh[b][:, h * 128:(h + 1) * 128], id16[:])
                for h in range(4):
                    K.tr(pb[:, 512 + h * 128:512 + (h + 1) * 128], kh[t % 3][:, h * 128:(h + 1) * 128], id16[:])
                evq = "act" if t % 2 == 0 else "dve"
                K.copy(evq, qT[b][:], pb[:, 0:512])
                K.copy(evq, kT[b][:], pb[:, 512:1024])

            def stage_B2(t):
                b = t % 2
                psc = PF[3]
                for h in range(4):
                    K.mm(psc[:, h * 128:(h + 1) * 128], kT[b][:, h * 128:(h + 1) * 128], qT[b][:, h * 128:(h + 1) * 128], True, True)
                K.tt("dve", pT[b][:], psc[:], mask[:], ALU.mult)

            def stage_B3(t):
                b = t % 2
                b3 = t % 3
                po = PF[4]
                for h in range(4):
                    hs = slice(h * 128, (h + 1) * 128)
                    K.mm(po[:, hs], pT[b][:, hs], vb[b3][:, hs], True, False)
                    K.mm(po[:, hs], qT[b][:, hs], Pb[:, h, :], False, True)
                kv_update(b3)
                for h in range(4):
                    K.S.add("dve", lambda e, h=h: e.bn_stats(out=st6[:, h, :], in_=po[:, h * 128:(h + 1) * 128]),
                            reads=[po], writes=[st6])
                for h in range(4):
                    K.S.add("dve", lambda e, h=h: e.bn_aggr(out=mv[:, h, :], in_=st6[:, h, :]),
                            reads=[st6], writes=[mv])
                rsqrt_pool(rs4[:], mv[:, :, 1], 1.0, 4)
                for h in range(4):
                    hs = slice(h * 128, (h + 1) * 128)
                    K.ts("dve", yn[:, hs], po[:, hs], mv[:, h, 0:1], ALU.subtract, rs4[:, h:h + 1], ALU.mult)
                K.tt("pool", yn[:], yn[:], gnw[:], ALU.mult)
                K.tt("pool", retb[b][:], yn[:], gs[b3][:], ALU.mult)

            def stage_D1(t):
                b = t % 2
                pb = PB[cnt["tr"] % 2]
                cnt["tr"] += 1
                for h in range(4):
                    K.tr(pb[:, h * 128:(h + 1) * 128], retb[b][:, h * 128:(h + 1) * 128], id16[:])
                K.copy("act", retT[b][:], pb[:, 0:512])
                K.dma("sync", xres[b][:], x_own[(t - 16) * 128:(t - 15) * 128, :], writes=[xres[b]])

            def stage_D2(t):
                ti = t - 16
                r0 = ti * 128
                b = t % 2
                cvb = convT[(t // 4) % 2]
                tix = t % 4
                if debug:
                    for nm, tt_ in (("kh", kh[t % 3]), ("vb", vb[t % 3]), ("qh", qh[b]), ("retb", retb[b])):
                        K.dma("sync", dbg[nm][r0:r0 + 128, :], tt_[:], reads=[tt_], writes=["dbg_" + nm], acc=True)
                x1t_ = xres[b]
                for half in range(2):
                    pw = next_pj()
                    for kc in range(8):
                        lhs = retT[b][:, kc * 128:(kc + 1) * 128] if kc < 4 else cvb[:, kc - 4, tix * 128:(tix + 1) * 128]
                        K.mm(pw[:], lhs, wob[:, kc, half * 512:(half + 1) * 512], kc == 0, kc == 7)
                    K.tt("dve", x1t_[:, half * 512:(half + 1) * 512], pw[:], x1t_[:, half * 512:(half + 1) * 512], ALU.add)
                K.dma("sync", x1_d[r0:r0 + 128, :], x1t_[:], reads=[x1t_], writes=["x1_d"], acc=True)
                if debug:
                    K.dma("sync", dbg["x1"][r0:r0 + 128, :], x1t_[:], reads=[x1t_], writes=["dbg_x1"], acc=True)
                K.act(junk[:], x1t_[:], AF.Square, accum=ssq2[:])
                rsqrt_pool(rstd2[:], ssq2[:], 1.0 / D, 1)
                K.stt(h2f[b][:], x1t_[:], rstd2[:, 0:1], n2w[:], ALU.mult, ALU.mult)
                K.copy("act", h2bt[b][:], h2f[b][:])
                K.dma("sync", h2_d[r0:r0 + 128, :], h2bt[b][:], reads=[h2bt[b]], writes=["h2_d"], acc=True)

            def stage_E1(t, rnd):
                b = t % 2
                p32 = PF[3]
                for jj in range(4):
                    j = rnd * 4 + jj
                    K.tr(p32[:, jj * 128:(jj + 1) * 128], h2f[b][:, j * 128:(j + 1) * 128], id32[:])
                K.copy("act", h2T[b][:, rnd * 4:(rnd + 1) * 4, :], p32[:].rearrange("p (j t) -> p j t", j=4), acc=(rnd > 0))

            def stage_E2(t):
                ti = t - 16
                plog = PF[5]
                for j in range(8):
                    K.mm(plog[:, 0:72], h2T[t % 2][:, j, :], wr[:, j, :], j == 0, j == 7)
                K.tt("dve", LG[:, ti, :], plog[:, 0:72], rb[:], ALU.add)

            own = lambda t: 16 <= t < 32
            for t in range(5):
                front_load(t)
            tab_load(0)
            for t in range(4):
                front(t)
            for s in range(32 + 5):
                nf = s + 4 if s + 4 < 32 else None
                if s + 5 < 32:
                    front_load(s + 5)
                if s + 1 < 32:
                    tab_load(s + 1)
                if nf is not None:
                    front_sq(nf)
                if s < 16:
                    for zi in range(4):
                        blk = s * 4 + zi
                        K.dma("sync", xs_d[blk * 128:(blk + 1) * 128, :], zt[:], reads=[zt], writes=["xs_d"], acc=True)
                    prefix_work(s)
                    if nf is not None:
                        front_hb(nf)
                        front_tr(nf)
                    if s >= 1:
                        kv_update((s - 1) % 3, pb_out=False)
                    if s == 15:
                        kv_update(15 % 3, pb_out=True)
                        for cc in range(4):
                            cu_chunk(3, cc)
                    continue
                if own(s - 2):
                    stage_B2(s - 2)
                if own(s):
                    if s % 4 == 0:
                        conv_group(s // 4)
                    stage_A(s)
                if nf is not None:
                    front_hb(nf)
                if own(s - 3):
                    stage_D1(s - 3)
                if own(s - 2):
                    stage_B3(s - 2)
                if own(s - 4):
                    stage_E1(s - 4, 0)
                if own(s - 1):
                    stage_B1(s - 1)
                if own(s - 4):
                    stage_E1(s - 4, 1)
                if own(s - 3):
                    stage_D2(s - 3)
                if own(s - 4):
                    stage_E2(s - 4)
                if nf is not None:
                    front_tr(nf)
            if debug:
                K.dma("sync", dbg["lg"], LG[:].rearrange("p t n -> p (t n)"), reads=[LG], writes=["dbg_lg"])
            S.emit(p1)

        with ExitStack() as p2:
            if phases < 2:
                return nc
            S = Sched(nc)
            S.all_selfwait = True
            K.S = S
            sb2 = lambda name, shape, dt: sb(name, shape, dt, p2)
            ustr = sb2("ustr", [128, 128], F32)
            ones = sb2("ones", [128, 128], F32)
            base = sb2("base", [128, 64], F32)
            m16 = sb2("m16", [128, NT], F32)
            ohg = sb2("ohg", [128, NT, 8], F32)
            sg = sb2("sg", [128, NT, 8], F32)
            se = sb2("se", [128, NT], F32)
            pg = sb2("pg", [128, NT], F32)
            tmp4 = sb2("tmp4", [128, NT, 8, 8], F32)
            sel = sb2("sel", [128, NT, 8], F32)
            sel2 = sb2("sel2", [128, NT, 8], F32)
            m1 = sb2("m1", [128, NT], F32)
            m2 = sb2("m2", [128, NT], F32)
            oh1 = sb2("oh1", [128, NT, 8], F32)
            oh2 = sb2("oh2", [128, NT, 8], F32)
            dd = sb2("dd", [128, NT], F32)
            OH = [sb2("OH%d" % k, [128, NT, 64], F32) for k in range(2)]
            Osum = sb2("Osum", [128, NT, 64], F32)
            Ccum = sb2("Ccum", [128, NT, 64], F32)
            Rk = sb2("Rk", [128, NT, 64], F32)
            dstf = sb2("dstf", [128, NT], F32)
            hsc = [sb2("hsc%d" % i, [128, D], BF16) for i in range(6)]
            NB = 5
            wg = [sb2("wg%d" % i, [128, 8, 512], BF16) for i in range(NB)]
            wu = [sb2("wu%d" % i, [128, 8, 512], BF16) for i in range(NB)]
            wd = [sb2("wd%d" % i, [128, 4, D], BF16) for i in range(NB)]
            xb = [sb2("xb%d" % i, [128, D], BF16) for i in range(2)]
            xT = [sb2("xT%d" % i, [128, 8, 128], BF16) for i in range(2)]
            gsl = [sb2("gsl%d" % i, [128, 512], F32) for i in range(2)]
            hT2 = [sb2("hT2%d" % i, [128, 512], BF16) for i in range(2)]
            yb = [sb2("yb%d" % i, [128, D], F32) for i in range(2)]

            def load_w(e_):
                wb_ = e_ % NB
                K.dma("pool", wg[wb_][:], w_gate[e_].rearrange("(j p) n -> p j n", p=128), writes=[wg[wb_]])
                K.dma("pool", wu[wb_][:], w_up[e_].rearrange("(j p) n -> p j n", p=128), writes=[wu[wb_]])
                K.dma("pool", wd[wb_][:], w_down[e_].rearrange("(j p) n -> p j n", p=128), writes=[wd[wb_]])

            if phases >= 3:
                load_w(0)
            for i in range(6):
                K.dma("sync", hsc[i][:], h2_d[i * 128:(i + 1) * 128, :], reads=["h2_d"], writes=[hsc[i]])
            K.dma("sync", ustr[:], c_ustr, writes=[ustr])
            K.dma("sync", ones[:], c_ones, writes=[ones])
            K.dma("sync", base[:], c_base, writes=[base])
            gl = LG[:, :, 0:8]
            el = LG[:, :, 8:72].rearrange("p t (g e) -> p t g e", g=8)
            bc3 = lambda a: a.unsqueeze(2).to_broadcast([128, NT, 8])
            K.red("dve", m16[:], gl, ALU.max)
            K.tt("dve", ohg[:], gl, bc3(m16[:]), ALU.is_equal)
            K.tt("dve", sg[:], gl, bc3(m16[:]), ALU.subtract)
            K.act(sg[:], sg[:], AF.Exp)
            K.red("dve", se[:], sg[:], ALU.add)
            K.recip(pg[:], se[:])
            K.tt("dve", tmp4[:], el, ohg[:].unsqueeze(3).to_broadcast([128, NT, 8, 8]), ALU.mult)
            K.red("dve", sel[:], tmp4[:].rearrange("p t g e -> p t e g"), ALU.add)
            K.red("dve", m1[:], sel[:], ALU.max)
            K.tt("dve", oh1[:], sel[:], bc3(m1[:]), ALU.is_equal)
            K.stt(sel2[:], oh1[:], NEG, sel[:], ALU.mult, ALU.add)
            K.red("dve", m2[:], sel2[:], ALU.max)
            K.tt("dve", oh2[:], sel2[:], bc3(m2[:]), ALU.is_equal)
            K.tt("dve", dd[:], m2[:], m1[:], ALU.subtract)
            K.act(dd[:], dd[:], AF.Exp)
            K.ts("dve", dd[:], dd[:], 1.0, ALU.add)
            K.recip(dd[:], dd[:])
            K.tt("dve", gw[0][:], pg[:], dd[:], ALU.mult)
            K.tt("dve", gw[1][:], pg[:], gw[0][:], ALU.subtract)
            for k, oh in ((0, oh1), (1, oh2)):
                K.tt("dve", OH[k][:].rearrange("p t (g e) -> p t g e", g=8),
                     ohg[:].unsqueeze(3).to_broadcast([128, NT, 8, 8]),
                     oh[:].unsqueeze(2).to_broadcast([128, NT, 8, 8]), ALU.mult)
            K.tt("dve", Osum[:], OH[0][:], OH[1][:], ALU.add)
            K.memset("dve", Ccum[:, 0, :], 0.0)
            for i in range(1, NT):
                K.tt("dve", Ccum[:, i, :], Ccum[:, i - 1, :], Osum[:, i - 1, :], ALU.add)
            for i in range(NT):
                pr = PF[i // 8]
                o0 = (i % 8) * 64
                K.mm(pr[:, o0:o0 + 64], ustr[:], Osum[:, i, :], True, False)
                K.mm(pr[:, o0:o0 + 64], ones[:], Ccum[:, i, :], False, True)
            for hf in range(2):
                K.tt("dve", Rk[:, hf * 8:(hf + 1) * 8, :], PF[hf][:].rearrange("p (t n) -> p t n", t=8),
                     base[:].unsqueeze(1).to_broadcast([128, 8, 64]), ALU.add)
            for k in range(2):
                K.tt("dve", OH[k][:], OH[k][:], Rk[:], ALU.mult)
                K.red("dve", dstf[:], OH[k][:], ALU.add)
                K.copy("dve", dst[k][:], dstf[:])
            if debug:
                rt = sb2("rtdbg", [128, 4 * NT], F32)
                K.copy("dve", rt[:, 0:NT], dst[0][:])
                K.copy("dve", rt[:, NT:2 * NT], dst[1][:])
                K.copy("dve", rt[:, 2 * NT:3 * NT], gw[0][:])
                K.copy("dve", rt[:, 3 * NT:4 * NT], gw[1][:])
                K.dma("sync", dbg["rt"], rt[:], reads=[rt], writes=["dbg_rt"])
            for i in range(NT):
                hb_ = hsc[i % 6]
                if i >= 6:
                    K.dma("sync", hb_[:], h2_d[i * 128:(i + 1) * 128, :], reads=["h2_d"], writes=[hb_])
                for k in range(2):
                    K.S.add("pool", lambda e, i=i, k=k, hb_=hb_: e.indirect_dma_start(
                        out=xs_d, out_offset=bass.IndirectOffsetOnAxis(ap=dst[k][:, i:i + 1], axis=0),
                        in_=hb_[:], in_offset=None, bounds_check=bnd(e), oob_is_err=False),
                        reads=[hb_, dst[k]], writes=["xs_d"], dma=True, acc=True)
            if phases >= 3:
                for e_ in range(1, NB):
                    load_w(e_)
                for e_ in range(NE):
                    wb_ = e_ % NB
                    b = e_ % 2
                    if e_ >= NB:
                        load_w(e_)
                    K.dma("sync", xb[b][:], xs_d[e_ * 128:(e_ + 1) * 128, :], reads=["xs_d"], writes=[xb[b]])
                    pb = PB[e_ % 2]
                    pv = pb[:].rearrange("p (j t) -> p j t", j=8)
                    for j in range(8):
                        K.tr(pv[:, j, :], xb[b][:, j * 128:(j + 1) * 128], id16[:])
                    K.copy("act" if e_ % 2 == 0 else "dve", xT[b][:], pv)
                    pgt = PF[0 + 2 * b]
                    put = PF[1 + 2 * b]
                    for w_, pf in ((wg[wb_], pgt), (wu[wb_], put)):
                        for f in range(4):
                            for j in range(8):
                                K.mm(pf[:, f * 128:(f + 1) * 128], w_[:, j, f * 128:(f + 1) * 128], xT[b][:, j, :], j == 0, j == 7)
                    K.act(gsl[b][:], pgt[:], AF.Silu)
                    K.tt("dve", hT2[b][:], gsl[b][:], put[:], ALU.mult)
                    for half in range(2):
                        py = PF[4 + half]
                        for f in range(4):
                            K.mm(py[:], hT2[b][:, f * 128:(f + 1) * 128], wd[wb_][:, f, half * 512:(half + 1) * 512], f == 0, f == 3)
                    K.copy("act", yb[b][:, 0:512], PF[4][:])
                    K.copy("dve", yb[b][:, 512:1024], PF[5][:], acc=True)
                    K.dma("act", ys_d[e_ * 128:(e_ + 1) * 128, :], yb[b][:], reads=[yb[b]], writes=["ys_d"], acc=True)

            S.emit(p2)


        with ExitStack() as p4:
            if phases < 4:
                return nc
            S = Sched(nc)
            K.S = S
            sb4 = lambda name, shape, dt: sb(name, shape, dt, p4)
            fnw = sb4("fnw", [128, D], F32)
            NB4 = 4
            y0 = [sb4("y0_%d" % i, [128, D], F32) for i in range(NB4)]
            y1 = [sb4("y1_%d" % i, [128, D], F32) for i in range(NB4)]
            xo = [sb4("xo%d" % i, [128, D], F32) for i in range(NB4)]
            ot = [sb4("ot%d" % i, [128, D], F32) for i in range(2)]
            ssq4 = [sb4("ssq4_%d" % i, [128, 1], F32) for i in range(NB4)]
            rst4 = [sb4("rst4_%d" % i, [128, 1], F32) for i in range(NB4)]
            K.dma("sync", fnw[:], c_fnw, writes=[fnw])

            def p4_load(i):
                b = i % NB4
                r0 = i * 128
                K.dma("sync", xo[b][:], x1_d[r0:r0 + 128, :], reads=["x1_d"], writes=[xo[b]])
                for k, yk in ((0, y0[b]), (1, y1[b])):
                    K.S.add("pool", lambda e, i=i, k=k, yk=yk: e.indirect_dma_start(
                        out=yk[:], out_offset=None, in_=ys_d,
                        in_offset=bass.IndirectOffsetOnAxis(ap=dst[k][:, i:i + 1], axis=0),
                        bounds_check=bnd(e), oob_is_err=False),
                        reads=["ys_d", dst[k]], writes=[yk], dma=True)

            def p4_mid(i):
                b = i % NB4
                K.stt(xo[b][:], y0[b][:], gw[0][:, i:i + 1], xo[b][:], ALU.mult, ALU.add)
                K.stt(xo[b][:], y1[b][:], gw[1][:, i:i + 1], xo[b][:], ALU.mult, ALU.add)
                K.act(junk[:], xo[b][:], AF.Square, accum=ssq4[b][:])
                rsqrt_pool(rst4[b][:], ssq4[b][:], 1.0 / D, 1)

            def p4_fin(i):
                b = i % NB4
                r0 = i * 128
                K.stt(ot[i % 2][:], xo[b][:], rst4[b][:, 0:1], fnw[:], ALU.mult, ALU.mult)
                K.dma("act", y_out[r0:r0 + 128, :], ot[i % 2][:], reads=[ot[i % 2]], writes=["y_out"], acc=True)

            p4_load(0)
            p4_load(1)
            for s_ in range(NT + 1):
                if s_ + 2 < NT:
                    p4_load(s_ + 2)
                if s_ < NT:
                    p4_mid(s_)
                if 0 <= s_ - 1 < NT:
                    p4_fin(s_ - 1)
            S.emit(p4)
    return nc


def _tables():
    half = 64
    inv = (10000.0 ** (-np.arange(half, dtype=np.float32) / half)).astype(np.float32)
    pos = np.arange(SEQ, dtype=np.float32)
    ang = (pos[:, None] * inv[None, :]).astype(np.float32).astype(np.float64)
    cos = np.cos(ang)
    sin = np.sin(ang)
    logg = np.log1p(-(2.0 ** (-5.0 - np.arange(4, dtype=np.float64))))
    i = (np.arange(SEQ) % 128).astype(np.float64)
    dq = np.exp(logg[None, :] * (i[:, None] + 1.0 - 128.0)) * (128.0 ** -0.5)
    dk = np.exp(logg[None, :] * (127.0 - i[:, None]))
    tq_c = dq[:, :, None] * cos[:, None, :]
    tq_s = dq[:, :, None] * sin[:, None, :]
    tk_c = dk[:, :, None] * cos[:, None, :]
    tk_s = dk[:, :, None] * sin[:, None, :]
    full = np.stack([tq_c, tq_s, tk_c, tk_s], axis=1).reshape(SEQ, 4, 256).astype(np.float32)
    return full


def _prep(inputs):
    f = lambda a: np.ascontiguousarray(np.asarray(a, dtype=np.float32))
    x = f(inputs["x"])
    full = _tables()
    rep = lambda v: np.ascontiguousarray(np.broadcast_to(f(v).reshape(1, -1), (128, f(v).size)))
    wr = np.concatenate([f(inputs["router_g_w"])[0]] + [f(inputs["router_e_w"])[0, g] for g in range(8)], axis=1)
    rbias = np.concatenate([f(inputs["router_g_b"])[0].reshape(-1), f(inputs["router_e_b"])[0].reshape(-1)])
    cwv = f(inputs["conv_w"])[0]
    cw = np.ascontiguousarray(cwv.reshape(3, 4, 128).transpose(2, 1, 0).reshape(128, 12))
    jj, ii = np.meshgrid(np.arange(128), np.arange(128), indexing="ij")
    maskT = (ii >= jj).astype(np.float32)
    common = {
        "w_in": f(inputs["w_in"])[0], "w_o": f(inputs["w_o"])[0],
        "w_gate": f(inputs["w_gate"])[0], "w_up": f(inputs["w_up"])[0], "w_down": f(inputs["w_down"])[0],
        "wr": np.ascontiguousarray(wr),
        "c_n1w": rep(inputs["norm1_w"]), "c_n2w": rep(inputs["norm2_w"]), "c_fnw": rep(inputs["final_norm_w"]),
        "c_gnw": rep(inputs["ret_gn_w"]), "c_rb": rep(rbias), "c_cw": cw,
        "c_id16": np.eye(128).astype(ml_dtypes.bfloat16), "c_id32": np.eye(128, dtype=np.float32),
        "c_mask": np.ascontiguousarray(np.tile(maskT, (1, 4))),
        "c_ustr": (jj < ii).astype(np.float32),
        "c_ones": np.ones((128, 128), np.float32),
        "c_base": np.ascontiguousarray(np.broadcast_to((np.arange(64, dtype=np.float32) * 128.0)[None, :], (128, 64))),
    }
    in_maps = []
    for c in range(NCORES):
        b, hf = c // 2, c % 2
        m = dict(common)
        m["x_own"] = np.ascontiguousarray(x[b, hf * TOK:(hf + 1) * TOK])
        m["x_pre"] = np.ascontiguousarray(x[b, 0:TOK]) if hf == 1 else np.zeros((TOK, D), np.float32)
        m["tab_own"] = np.ascontiguousarray(full[hf * TOK:(hf + 1) * TOK])
        m["tab_pre"] = np.ascontiguousarray(full[0:TOK, 2:4])
        in_maps.append(m)
    return in_maps


_NC_CACHE = {}


def kernel(**inputs):
    in_maps = _prep(inputs)
    if "nc" not in _NC_CACHE:
        _NC_CACHE["nc"] = build_nc(0)
    res = run_bass_kernel_spmd(_NC_CACHE["nc"], in_maps, core_ids=list(range(NCORES)))
    out = np.empty((4, SEQ, D), np.float32)
    for c in range(NCORES):
        b, hf = c // 2, c % 2
        out[b, hf * TOK:(hf + 1) * TOK] = res.results[c]["y_out"]
    return out
```
